# Optimizing a Trainium2 kernel written in Bass

```python
import jax, jax.numpy as jnp
from jax import lax
import numpy as np

D_MODEL = 1024
BATCH = 16
SEQ = 2048
DEPTH = 1

N_MEM = 256
XATTN_HEADS = 4
XATTN_HEAD_DIM = D_MODEL // XATTN_HEADS
CONV_WIDTH = D_MODEL // 2
CONV_K = 3
ATTN_HEADS = 8
HEAD_DIM = 64
ATTN_WIDTH = ATTN_HEADS * HEAD_DIM
MOBA_BLOCK = 256
MOBA_TOPK = 3
MOBA_Q_CHUNK = 32
ROPE_THETA = 10000.0
N_BRANCH = 2
IN_PROJ_WIDTH = 3 * CONV_WIDTH + 3 * ATTN_WIDTH + N_BRANCH * D_MODEL
PEER_HEADS = 8
PEER_N_KEYS = 128
PEER_N_EXPERTS = PEER_N_KEYS * PEER_N_KEYS
PEER_QUERY_DIM = 256
PEER_HALF = PEER_QUERY_DIM // 2
PEER_TOPK = 16
PEER_TOKEN_CHUNK = 128

EPS = 1e-6
MASK_VALUE = -1e30

kernel_name = "hybrid_conv_moba_xattn_peer_block"


def rmsnorm(x, g):
    xf = x.astype(jnp.float32)
    y = xf * lax.rsqrt(jnp.mean(xf * xf, axis=-1, keepdims=True) + EPS)
    return (y * g.astype(jnp.float32)).astype(x.dtype)


def rope(x):
    s, hd = x.shape[2], x.shape[3]
    half = hd // 2
    inv = ROPE_THETA ** (-jnp.arange(half, dtype=jnp.float32) / half)
    ang = jnp.arange(s, dtype=jnp.float32)[:, None] * inv[None, :]
    cos, sin = jnp.cos(ang), jnp.sin(ang)
    xf = x.astype(jnp.float32)
    x1, x2 = xf[..., :half], xf[..., half:]
    out = jnp.concatenate([x1 * cos - x2 * sin, x2 * cos + x1 * sin], axis=-1)
    return out.astype(x.dtype)


def short_gated_conv(xin, b_gate, c_gate, w_conv):
    u = c_gate * xin
    ch = u.shape[-1]
    y = lax.conv_general_dilated(
        u, w_conv.reshape(CONV_K, 1, ch), window_strides=(1,),
        padding=[(CONV_K - 1, 0)], dimension_numbers=("NWC", "WIO", "NWC"),
        feature_group_count=ch)
    return b_gate * y


def moba_attention(q, k, v):
    b, h, s, hd = q.shape
    n_blk = -(-s // MOBA_BLOCK)
    pad = n_blk * MOBA_BLOCK - s
    kb = jnp.pad(k, ((0, 0), (0, 0), (0, pad), (0, 0))).reshape(b, h, n_blk, MOBA_BLOCK, hd)
    vb = jnp.pad(v, ((0, 0), (0, 0), (0, pad), (0, 0))).reshape(b, h, n_blk, MOBA_BLOCK, hd)
    kbar = jnp.mean(kb.astype(jnp.float32), axis=3)
    q_blk = jnp.arange(s) // MOBA_BLOCK
    gate = jnp.einsum("bhsd,bhnd->bhsn", q.astype(jnp.float32), kbar)
    past = jnp.arange(n_blk)[None, :] < q_blk[:, None]
    gate = jnp.where(past, gate, MASK_VALUE)
    if n_blk > 1:
        _, sel = lax.top_k(gate, min(MOBA_TOPK, n_blk - 1))
    else:
        sel = jnp.zeros((b, h, s, 1), dtype=jnp.int32)
    n_sel = sel.shape[-1]
    scale = hd ** -0.5
    n_chunk = s // MOBA_Q_CHUNK
    q_c = q.reshape(b, h, n_chunk, MOBA_Q_CHUNK, hd).transpose(2, 0, 1, 3, 4)
    sel_c = sel.reshape(b, h, n_chunk, MOBA_Q_CHUNK, n_sel).transpose(2, 0, 1, 3, 4)
    gather_blocks = jax.vmap(jax.vmap(lambda t, i: t[i]))

    def step(args):
        c, qc, selc = args
        start = c * MOBA_Q_CHUNK
        b_own = start // MOBA_BLOCK
        k_own = lax.dynamic_index_in_dim(kb, b_own, axis=2, keepdims=False)
        v_own = lax.dynamic_index_in_dim(vb, b_own, axis=2, keepdims=False)
        q_pos = start + jnp.arange(MOBA_Q_CHUNK)
        k_pos = b_own * MOBA_BLOCK + jnp.arange(MOBA_BLOCK)
        s_own = jnp.einsum("bhqd,bhkd->bhqk", qc, k_own).astype(jnp.float32) * scale
        s_own = jnp.where(k_pos[None, :] <= q_pos[:, None], s_own, MASK_VALUE)
        k_sel = gather_blocks(kb, selc)
        v_sel = gather_blocks(vb, selc)
        s_sel = jnp.einsum("bhqd,bhqnkd->bhqnk", qc, k_sel).astype(jnp.float32) * scale
        ok = selc < b_own
        s_sel = jnp.where(ok[..., None], s_sel, MASK_VALUE)
        scores = jnp.concatenate(
            [s_sel.reshape(b, h, MOBA_Q_CHUNK, n_sel * MOBA_BLOCK), s_own], axis=-1)
        p = jax.nn.softmax(scores, axis=-1).astype(v.dtype)
        p_sel = p[..., :n_sel * MOBA_BLOCK].reshape(b, h, MOBA_Q_CHUNK, n_sel, MOBA_BLOCK)
        p_own = p[..., n_sel * MOBA_BLOCK:]
        return (jnp.einsum("bhqnk,bhqnkd->bhqd", p_sel, v_sel)
                + jnp.einsum("bhqk,bhkd->bhqd", p_own, v_own))

    out = lax.map(step, (jnp.arange(n_chunk), q_c, sel_c))
    return out.transpose(1, 2, 0, 3, 4).reshape(b, h, s, hd)


def memory_cross_attention(a, mem_n, w_q, w_kv, w_o):
    b, s, d = a.shape
    q = (a @ w_q).reshape(b, s, XATTN_HEADS, XATTN_HEAD_DIM)
    k, v = jnp.split(mem_n @ w_kv, 2, axis=-1)
    k = k.reshape(b, -1, XATTN_HEADS, XATTN_HEAD_DIM)
    v = v.reshape(b, -1, XATTN_HEADS, XATTN_HEAD_DIM)
    scores = jnp.einsum("bshd,bmhd->bhsm", q, k).astype(jnp.float32) * (XATTN_HEAD_DIM ** -0.5)
    p = jax.nn.softmax(scores, axis=-1).astype(v.dtype)
    o = jnp.einsum("bhsm,bmhd->bshd", p, v).reshape(b, s, d)
    return o @ w_o


def peer_ffn(a, w_pq, sub_keys, u, v):
    b, s, d = a.shape
    q = (a @ w_pq).reshape(b, s, PEER_HEADS, 2, PEER_HALF)
    s_half = jnp.einsum("bshpd,hpnd->bshpn", q, sub_keys).astype(jnp.float32)
    v_half, i_half = lax.top_k(s_half, PEER_TOPK)
    cand = v_half[..., 0, :, None] + v_half[..., 1, None, :]
    cand_idx = i_half[..., 0, :, None] * PEER_N_KEYS + i_half[..., 1, None, :]
    cand = cand.reshape(b, s, PEER_HEADS, PEER_TOPK * PEER_TOPK)
    cand_idx = cand_idx.reshape(b, s, PEER_HEADS, PEER_TOPK * PEER_TOPK)
    top_s, pos = lax.top_k(cand, PEER_TOPK)
    expert = jnp.take_along_axis(cand_idx, pos, axis=-1)
    g = jax.nn.softmax(top_s, axis=-1).astype(a.dtype)
    n_tok = b * s
    n_chunk = n_tok // PEER_TOKEN_CHUNK
    n_sel = PEER_HEADS * PEER_TOPK
    x_c = a.reshape(n_chunk, PEER_TOKEN_CHUNK, d)
    e_c = expert.reshape(n_chunk, PEER_TOKEN_CHUNK, n_sel)
    g_c = g.reshape(n_chunk, PEER_TOKEN_CHUNK, n_sel)

    def step(args):
        xc, ec, gc = args
        act = jax.nn.gelu(jnp.einsum("cd,ced->ce", xc, u[ec]))
        return jnp.einsum("ce,ced->cd", gc * act, v[ec])

    return lax.map(step, (x_c, e_c, g_c)).reshape(b, s, d)


def setup_inputs(seed: int = 0) -> dict:
    key = jax.random.key(seed)
    ks = jax.random.split(key, 20)
    f32 = jnp.float32
    nrm = lambda k, shape, sc: jax.random.normal(k, shape, f32) * sc
    gain = lambda k, shape: 1.0 + 0.02 * jax.random.normal(k, shape, f32)
    L, D = DEPTH, D_MODEL
    return {
        "x": nrm(ks[0], (BATCH, SEQ, D), 1.0),
        "mem": nrm(ks[1], (BATCH, N_MEM, D), 1.0),
        "g_mix": gain(ks[2], (L, D)),
        "w_in": nrm(ks[3], (L, D, IN_PROJ_WIDTH), D ** -0.5),
        "w_conv": nrm(ks[4], (L, CONV_K, CONV_WIDTH), CONV_K ** -0.5),
        "w_conv_out": nrm(ks[5], (L, CONV_WIDTH, D), CONV_WIDTH ** -0.5),
        "w_attn_out": nrm(ks[6], (L, ATTN_WIDTH, D), ATTN_WIDTH ** -0.5),
        "w_merge": nrm(ks[7], (L, D, D), D ** -0.5),
        "g_xattn": gain(ks[8], (L, D)),
        "g_mem": gain(ks[9], (L, D)),
        "w_xq": nrm(ks[10], (L, D, D), D ** -0.5),
        "w_xkv": nrm(ks[11], (L, D, 2 * D), D ** -0.5),
        "w_xo": nrm(ks[12], (L, D, D), D ** -0.5),
        "g_ffn": gain(ks[13], (L, D)),
        "w_pq": nrm(ks[14], (L, D, PEER_HEADS * PEER_QUERY_DIM), D ** -0.5),
        "peer_sub_keys": nrm(ks[15], (L, PEER_HEADS, 2, PEER_N_KEYS, PEER_HALF), PEER_HALF ** -0.5),
        "peer_u": nrm(ks[16], (L, PEER_N_EXPERTS, D), D ** -0.5),
        "peer_v": nrm(ks[17], (L, PEER_N_EXPERTS, D), PEER_HEADS ** -0.5),
        "g_final": gain(ks[18], (D,)),
    }


def reference(x, mem, g_mix, w_in, w_conv, w_conv_out, w_attn_out, w_merge, g_xattn, g_mem,
              w_xq, w_xkv, w_xo, g_ffn, w_pq, peer_sub_keys, peer_u, peer_v, g_final):
    b, s, d = x.shape
    cw, aw = CONV_WIDTH, ATTN_WIDTH
    splits = [cw, 2 * cw, 3 * cw, 3 * cw + aw, 3 * cw + 2 * aw, 3 * cw + 3 * aw,
              3 * cw + 3 * aw + d]
    h = x
    for l in range(DEPTH):
        a = rmsnorm(h, g_mix[l])
        xin, b_gate, c_gate, q, k, v, gate_conv, gate_attn = jnp.split(a @ w_in[l], splits, axis=-1)
        y_conv = short_gated_conv(xin, b_gate, c_gate, w_conv[l]) @ w_conv_out[l]
        to_heads = lambda t: t.reshape(b, s, ATTN_HEADS, HEAD_DIM).transpose(0, 2, 1, 3)
        o = moba_attention(rope(to_heads(q)), rope(to_heads(k)), to_heads(v))
        y_attn = o.transpose(0, 2, 1, 3).reshape(b, s, aw) @ w_attn_out[l]
        merged = jax.nn.sigmoid(gate_conv) * y_conv + jax.nn.sigmoid(gate_attn) * y_attn
        h = h + merged @ w_merge[l]
        h = h + memory_cross_attention(rmsnorm(h, g_xattn[l]), rmsnorm(mem, g_mem[l]),
                                       w_xq[l], w_xkv[l], w_xo[l])
        h = h + peer_ffn(rmsnorm(h, g_ffn[l]), w_pq[l], peer_sub_keys[l], peer_u[l], peer_v[l])
    return rmsnorm(h, g_final)
```

```python
import contextlib
import numpy as np
import ml_dtypes
import concourse.bass as bass
import concourse.mybir as mybir
from concourse.bass_utils import run_bass_kernel_spmd

F32 = mybir.dt.float32
BF16 = mybir.dt.bfloat16
U32 = mybir.dt.uint32
U8 = mybir.dt.uint8
AF = mybir.ActivationFunctionType
ALU = mybir.AluOpType
AX = mybir.AxisListType

PE, ACT, DVE, POOL, SP = "tensor", "scalar", "vector", "gpsimd", "sync"
ENGS = [PE, ACT, DVE, POOL, SP]
N_DMA_SEMS = 24

D = 1024
SEQ = 2048
NMEM = 256
NEXP = 16384
TILE = 512
EPS = 1e-6
NEGB = 640.0


class Buf:
    __slots__ = ("name", "last_write", "reads", "excl")

    def __init__(self, name="", excl=False):
        self.name = name
        self.last_write = None
        self.reads = []
        self.excl = excl


class Op:
    __slots__ = ("eng", "fn", "deps", "is_dma", "signal", "sig_val", "dma_sem", "dma_val")

    def __init__(self, eng, fn, is_dma):
        self.eng = eng
        self.fn = fn
        self.is_dma = is_dma
        self.deps = []
        self.signal = False
        self.sig_val = None
        self.dma_sem = None
        self.dma_val = None


class Prog:
    def __init__(self, nc):
        self.nc = nc
        self.ops = []
        self.n_dma = {SP: 0, POOL: 0}
        self.last_eng = {}
        self.last_dma = {}
        self.bar_ops = []
        self.bar_pending = set()

    def barrier(self):
        self.bar_ops = list(self.last_eng.values()) + list(self.last_dma.values())
        self.bar_pending = set(ENGS)

    def op(self, eng, fn, reads=(), writes=(), dma=False):
        o = Op(eng, fn, dma)
        deps = []
        ex = [b for b in reads if b.excl]
        if ex:
            reads = [b for b in reads if not b.excl]
            writes = list(writes) + ex
        if eng in self.bar_pending:
            self.bar_pending.discard(eng)
            deps.extend(self.bar_ops)
        for b in reads:
            if b.last_write is not None:
                deps.append(b.last_write)
        for b in writes:
            if b.last_write is not None:
                deps.append(b.last_write)
            deps.extend(b.reads)
        seen = set()
        for d in deps:
            if d is o or id(d) in seen:
                continue
            seen.add(id(d))
            if (not d.is_dma) and (not dma) and d.eng == PE and eng == PE:
                continue
            o.deps.append(d)
            if not d.is_dma:
                d.signal = True
        for b in reads:
            if not dma:
                b.reads = [r for r in b.reads if r.is_dma or r.eng != eng]
            b.reads.append(o)
        for b in writes:
            b.last_write = o
            b.reads = []
        if dma:
            base, cnt = (0, 16) if eng == SP else (16, 8)
            k = self.n_dma[eng]
            o.dma_sem = base + k % cnt
            o.dma_val = 16 * (k // cnt + 1)
            self.n_dma[eng] = k + 1
            self.last_dma[o.dma_sem] = o
        else:
            self.last_eng[eng] = o
        self.ops.append(o)
        return o

    def emit(self):
        nc = self.nc
        cnt = {e: 0 for e in ENGS}
        for o in self.ops:
            if not o.is_dma and o.signal:
                cnt[o.eng] += 1
                o.sig_val = cnt[o.eng]
        with contextlib.ExitStack() as st:
            esem = {e: st.enter_context(nc.semaphore("s_" + e)) for e in ENGS}
            dsem = [st.enter_context(nc.semaphore("d_%d" % i)) for i in range(N_DMA_SEMS)]
            block = st.enter_context(nc.Block())
            per_eng = {e: [o for o in self.ops if o.eng == e] for e in ENGS}
            last_dma = {s: o.dma_val for s, o in self.last_dma.items()}

            def make(e):
                def body(eng):
                    waited = {}

                    def wait(sem, key, val):
                        if waited.get(key, 0) >= val:
                            return
                        waited[key] = val
                        eng.wait_ge(sem, val)

                    for o in per_eng[e]:
                        if o.is_dma and o.dma_val > 16:
                            wait(dsem[o.dma_sem], ("d", o.dma_sem), o.dma_val - 16)
                        for d in o.deps:
                            if d.is_dma:
                                wait(dsem[d.dma_sem], ("d", d.dma_sem), d.dma_val)
                            else:
                                wait(esem[d.eng], ("e", d.eng), d.sig_val)
                        ins = o.fn(eng)
                        if o.is_dma:
                            ins.then_inc(dsem[o.dma_sem], 16)
                        elif o.signal:
                            ins.then_inc(esem[o.eng], 1)
                    if e == SP:
                        for s, v in last_dma.items():
                            wait(dsem[s], ("d", s), v)
                return body

            for e in ENGS:
                getattr(block, e)(make(e))


_DT_SIZE = {F32: 4, BF16: 2, U32: 4, U8: 1}


class Arena:
    def __init__(self, nc, name, nbytes):
        self.t = nc.alloc_sbuf_tensor(name, [128, nbytes], U8)
        self.n = nbytes
        self.off = 0

    def alloc(self, shape, dt):
        sz = _DT_SIZE[dt]
        n = int(np.prod(shape)) * sz
        off = (self.off + 63) // 64 * 64
        assert off + n <= self.n, ("arena overflow", off, n, self.n)
        self.off = off + n
        v = self.t[:, off:off + n].bitcast(dt)
        if len(shape) == 2:
            v = v.rearrange("p (a b) -> p a b", a=shape[0])
        elif len(shape) == 3:
            v = v.rearrange("p (a b c) -> p a b c", a=shape[0], b=shape[1])
        return v


class _Stop(Exception):
    pass


def build_nc(NB, dbg=False, stop=None, skip_c=False):
    nc = bass.Bass("TRN2", target_bir_lowering=False)
    NT = NB * SEQ

    def din(name, shape, dt=F32):
        return nc.dram_tensor(name, shape, dt, kind="ExternalInput").ap()

    x_d = din("x", [NT, D])
    mem_d = din("mem", [NB * NMEM, D])
    win_d = din("w_in_ext", [D, 6144])
    wco_d = din("w_conv_out", [512, D])
    wao_d = din("w_attn_out", [512, D])
    wm_d = din("w_merge", [D, D])
    wxq_d = din("w_xq", [D, D])
    wxkv_d = din("w_xkv", [D, 2048])
    wxo_d = din("w_xo", [D, D])
    wpq_d = din("w_pq", [D, 2048])
    sk_d = din("sub_keys", [2048, 128])
    u_d = din("peer_u", [NEXP, D])
    v_d = din("peer_v", [NEXP, D])
    gtab_d = din("gtab", [128, 32])
    gfin_d = din("gfin_bc", [128, D])
    wconv_d = din("wconvT", [128, 12])
    cos_d = din("cosT", [128, SEQ])
    sin_d = din("sinT", [128, SEQ])
    past_d = din("past64", [SEQ, 64])
    neg_d = din("neg64", [SEQ, 64])
    own_d = din("own64", [SEQ, 64])
    tri_d = din("tri", [128, 128])
    eb_d = din("eb", [8, 1024])
    id_d = din("ident", [128, 128])
    iota_d = din("iota128", [128, 128])
    iota16_d = din("iota16", [128, 16])
    iorep_d = din("iorep", [128, 128 * 16])
    out_d = nc.dram_tensor("out", [NT, D], F32, kind="ExternalOutput").ap()
    dbg_d = {}
    if dbg:
        for nm in ("h1", "h2"):
            dbg_d[nm] = nc.dram_tensor("dbg_" + nm, [NT // TILE, 128, 8 * TILE], F32,
                                       kind="ExternalOutput").ap()
        dbg_d["pe"] = nc.dram_tensor("dbg_pe", [NT, D], F32, kind="ExternalOutput").ap()
        dbg_d["bias"] = nc.dram_tensor("dbg_bias", [NT, 64], BF16, kind="ExternalOutput").ap()
        dbg_d["gate"] = nc.dram_tensor("dbg_gate", [NT, 64], F32, kind="ExternalOutput").ap()
        dbg_d["o"] = nc.dram_tensor("dbg_o", [NT, 512], BF16, kind="ExternalOutput").ap()

    uT_s = nc.dram_tensor("uT_s", [128, 8, NEXP], BF16).ap()
    v_s = nc.dram_tensor("v_s", [NEXP, D], BF16).ap()
    win_s = nc.dram_tensor("win_s", [D, 6144], BF16).ap()
    wco_s = nc.dram_tensor("wco_s", [512, D], BF16).ap()
    wao_s = nc.dram_tensor("wao_s", [512, D], BF16).ap()
    wm_s = nc.dram_tensor("wm_s", [D, D], BF16).ap()
    wxq_s = nc.dram_tensor("wxq_s", [D, D], BF16).ap()
    wxkv_s = nc.dram_tensor("wxkv_s", [D, 2048], BF16).ap()
    wxo_s = nc.dram_tensor("wxo_s", [D, D], BF16).ap()
    wpq_s = nc.dram_tensor("wpq_s", [D, 2048], BF16).ap()
    mkT_s = nc.dram_tensor("mkT_s", [128, 8 * NMEM], BF16).ap()
    mv_s = nc.dram_tensor("mv_s", [128, 2 * D], BF16).ap()

    P = Prog(nc)

    def sbt(name, shape, dt):
        return nc.alloc_sbuf_tensor(name, shape, dt)

    ident32 = sbt("ident32", [128, 128], F32)
    identb = sbt("identb", [128, 128], BF16)
    onesb = sbt("onesb", [128, 128], BF16)
    iota128 = sbt("iota128s", [128, 128], F32)
    iota16 = sbt("iota16s", [128, 16], F32)
    trib = sbt("trib", [128, 128], BF16)
    gtab = sbt("gtabs", [128, 32], F32)
    wconv = sbt("wconvs", [128, 12], F32)
    ebb = sbt("ebb", [8, 8, 128], BF16)
    hT = sbt("hT", [128, 8, TILE], F32)
    KT = sbt("KT", [128, 4, SEQ], BF16)
    V = sbt("V", [128, 16, 8, 65], BF16)
    kbarT = sbt("kbarT", [128, 4, 8], BF16)
    uhalo = sbt("uhalo", [128, 4, 2], F32)
    B_const = Buf("const")
    B_hT = Buf("hT")
    B_KT = Buf("KT")
    B_V = Buf("V")
    B_kbar = Buf("kbar")
    B_uh = Buf("uhalo")
    B_out = Buf("out")
    B_uTs = Buf("uT_s")
    B_vs = Buf("v_s")
    B_mks = Buf("mk_s")
    B_mvs = Buf("mv_s")

    ARENA_BYTES = 141 * 1024
    arena = Arena(nc, "arena", ARENA_BYTES)
    banks = [nc.alloc_psum_tensor("pb%d" % i, [128, 512], F32) for i in range(8)]
    B_bank = [Buf("pb%d" % i, excl=True) for i in range(8)]

    def bank_bf(i):
        return banks[i][:].bitcast(BF16)

    def mm(out, lhsT, rhs, start, stop, r, w, skip=False):
        if skip:
            P.op(PE, lambda e: e.matmul(out, lhsT, rhs, start=start, stop=stop,
                                        skip_group_check=True), reads=r, writes=w)
        else:
            P.op(PE, lambda e: e.matmul(out, lhsT, rhs, start=start, stop=stop), reads=r, writes=w)

    def tr(out, in_, ident, r, w):
        P.op(PE, lambda e: e.transpose(out, in_, ident), reads=r, writes=w)

    def act(out, in_, func, r, w, bias=None, scale=None, accum=None):
        kw = {}
        if bias is not None:
            kw["bias"] = bias
        if scale is not None:
            kw["scale"] = scale
        if accum is not None:
            kw["accum_out"] = accum
        P.op(ACT, lambda e: e.activation(out, in_, func, **kw), reads=r, writes=w)

    def tt(eng, out, in0, in1, op, r, w):
        P.op(eng, lambda e: e.tensor_tensor(out, in0, in1, op), reads=r, writes=w)

    def ts(eng, out, in0, s1, s2, op0, op1, r, w):
        if op1 is None:
            P.op(eng, lambda e: e.tensor_scalar(out, in0, s1, None, op0), reads=r, writes=w)
        else:
            P.op(eng, lambda e: e.tensor_scalar(out, in0, s1, s2, op0, op1), reads=r, writes=w)

    def stt(eng, out, in0, scalar, in1, op0, op1, r, w):
        P.op(eng, lambda e: e.scalar_tensor_tensor(out, in0, scalar, in1, op0, op1), reads=r, writes=w)

    def cp(eng, out, in_, r, w):
        if eng == ACT:
            P.op(ACT, lambda e: e.activation(out, in_, AF.Copy), reads=r, writes=w)
        else:
            P.op(eng, lambda e: e.tensor_copy(out, in_), reads=r, writes=w)

    def dma(eng, out, in_, r, w):
        P.op(eng, lambda e: e.dma_start(out=out, in_=in_), reads=r, writes=w, dma=True)

    def memset(eng, ap, val, w):
        P.op(eng, lambda e: e.memset(ap, val), writes=w)

    dma(SP, ident32[:], id_d, [], [B_const])
    dma(POOL, identb[:], id_d, [], [B_const])
    dma(SP, iota128[:], iota_d, [], [B_const])
    dma(SP, iota16[:], iota16_d, [], [B_const])
    dma(POOL, trib[:], tri_d, [], [B_const])
    dma(SP, gtab[:], gtab_d, [], [B_const])
    dma(SP, wconv[:], wconv_d, [], [B_const])
    dma(POOL, ebb[:], eb_d.rearrange("n (b k) -> n b k", b=8), [], [B_const])
    memset(DVE, onesb[:], 1.0, [B_const])
    memset(DVE, V[:], 1.0, [B_V])

    for (dst, src, rows) in ((wxkv_s, wxkv_d, D), (win_s, win_d, D), (wco_s, wco_d, 512), (wao_s, wao_d, 512),
                             (wm_s, wm_d, D), (wxq_s, wxq_d, D), (wxo_s, wxo_d, D), (wpq_s, wpq_d, D)):
        for r0 in range(0, rows, 256):
            dma(POOL, dst[r0:r0 + 256, :], src[r0:r0 + 256, :], [], [])
    ub = [sbt("ub%d" % i, [128, D], BF16) for i in range(2)]
    B_ub = [Buf("ub0"), Buf("ub1")]
    uts = [sbt("uts%d" % i, [128, 8, 128], BF16) for i in range(2)]
    B_uts = [Buf("uts0"), Buf("uts1")]
    prep_state = [0]

    def prep_slice(n):
        while n > 0 and prep_state[0] < NEXP // 128:
            i = prep_state[0]
            prep_state[0] += 1
            n -= 1
            s = i % 2
            if i % 16 == 0:
                r0 = (i // 16) * 2048
                dma(POOL, v_s[r0:r0 + 2048, :], v_d[r0:r0 + 2048, :], [], [])
            dma(POOL, ub[s][:], u_d[i * 128:(i + 1) * 128, :], [], [B_ub[s]])
            bk = 2 + i % 2
            pv = bank_bf(bk)
            for kc in range(8):
                tr(pv[:, kc * 128:(kc + 1) * 128], ub[s][:, kc * 128:(kc + 1) * 128], identb[:],
                   [B_ub[s], B_const], [B_bank[bk]])
            cp(ACT if i % 2 == 0 else DVE, uts[s][:],
               pv.rearrange("p (a b) -> p a b", a=8), [B_bank[bk]], [B_uts[s]])
            dma(SP, uT_s[:, :, i * 128:(i + 1) * 128], uts[s][:], [B_uts[s]], [])

    arena.off = 0
    prep_slice(4)

    skT = sbt("skT", [128, 16, 128], BF16)
    B_skT = Buf("skT")
    skl = arena.alloc([16, 128], BF16)
    B_skl = Buf("skl")
    dma(POOL, skl, sk_d.rearrange("(c n) d -> n c d", n=128), [], [B_skl])
    for c in range(16):
        bk = 2 + c // 8
        pv = bank_bf(bk)
        tr(pv[:, (c % 8) * 128:(c % 8 + 1) * 128], skl[:, c, :], identb[:], [B_skl, B_const], [B_bank[bk]])
        if c % 8 == 7:
            cp(DVE, skT[:, c - 7:c + 1, :], pv.rearrange("p (a b) -> p a b", a=8), [B_bank[bk]], [B_skT])

    def norm_fm(src, B_src, N, gcol0, aT, B_aT, sq, B_sq, rs1, rs2, B_rs, bk):
        for kc in range(8):
            s = kc % 2
            act(sq[s][:, :N], src[:, kc, :N], AF.Square, [B_src], [B_sq[s]])
            mm(banks[bk][:, :N], onesb[:], sq[s][:, :N], kc == 0, kc == 7, [B_sq[s], B_const], [B_bank[bk]])
        act(rs1[:, :N], banks[bk][:, :N], AF.Sqrt, [B_bank[bk]], [B_rs[0]], bias=EPS, scale=1.0 / D)
        P.op(DVE, lambda e: e.reciprocal(rs2[:, :N], rs1[:, :N]), reads=[B_rs[0]], writes=[B_rs[1]])
        for kc in range(8):
            stt(DVE, aT[:, kc, :N], src[:, kc, :N],
                gtab[:, gcol0 + kc:gcol0 + kc + 1], rs2[:, :N], ALU.mult, ALU.mult,
                [B_src, B_rs[1], B_const], [B_aT])

    class WStream:
        def __init__(self, nbuf, kc, ncols):
            self.bufs = [arena.alloc([kc, ncols], BF16) for _ in range(nbuf)]
            self.B = [Buf("w%d" % i) for i in range(nbuf)]
            self.i = 0

        def load(self, src, kc):
            s = self.i % len(self.bufs)
            self.i += 1
            dma(POOL, self.bufs[s][:, :kc, :], src.rearrange("(kc p) n -> p kc n", p=128), [], [self.B[s]])
            return self.bufs[s], self.B[s]

    def proj_fm(w, B_w, col0, aT, B_aT, N, bk, kcs=8):
        for kc in range(kcs):
            mm(banks[bk][:, :N], w[:, kc, col0:col0 + 128], aT[:, kc, :N], kc == 0, kc == kcs - 1,
               [B_w, B_aT], [B_bank[bk]])

    def chk(k):
        if stop == k:
            raise _Stop()

    try:
     chk(0)
     for bi in range(NB):
         P.barrier()
         arena.off = 0
         ws = WStream(3, 8, 512)
         memT = arena.alloc([8, NMEM], F32)
         B_memT = Buf("memT")
         mtm = [arena.alloc([D], F32) for _ in range(2)]
         B_mtm = [Buf("mtm0"), Buf("mtm1")]
         mnT = arena.alloc([8, NMEM], BF16)
         B_mnT = Buf("mnT")
         sq = [arena.alloc([TILE], BF16) for _ in range(2)]
         B_sq = [Buf("sq0"), Buf("sq1")]
         rs1 = arena.alloc([TILE], F32)
         rs2 = arena.alloc([TILE], F32)
         B_rs = [Buf("rs1"), Buf("rs2")]
         mk_sb = arena.alloc([8, NMEM], BF16)
         B_mk = Buf("mk_sb")
         mv_sb = arena.alloc([2, D], BF16)
         B_mv = Buf("mv_sb")
         for sub in range(2):
             r0 = bi * NMEM + sub * 128
             dma(SP, mtm[sub], mem_d[r0:r0 + 128, :], [], [B_mtm[sub]])
             for half in range(2):
                 bk = half
                 for j in range(4):
                     kc = half * 4 + j
                     tr(banks[bk][:, j * 128:(j + 1) * 128], mtm[sub][:, kc * 128:(kc + 1) * 128], ident32[:],
                        [B_mtm[sub], B_const], [B_bank[bk]])
                 cp(ACT if half == 0 else DVE, memT[:, half * 4:half * 4 + 4, sub * 128:(sub + 1) * 128],
                    banks[bk][:].rearrange("p (a b) -> p a b", a=4), [B_bank[bk]], [B_memT])
         norm_fm(memT, B_memT, NMEM, 16, mnT, B_mnT, sq, B_sq, rs1, rs2, B_rs, 2)
         for g in range(4):
             w, B_w = ws.load(wxkv_s[:, g * 512:(g + 1) * 512], 8)
             if g < 2:
                 for cc in range(4):
                     c = g * 4 + cc
                     bk = c % 2
                     proj_fm(w, B_w, cc * 128, mnT, B_mnT, NMEM, bk)
                     cp(ACT if c % 2 == 0 else DVE, mk_sb[:, c, :], banks[bk][:, :NMEM], [B_bank[bk]], [B_mk])
             else:
                 for mc in range(2):
                     bk = 2 + mc
                     for kc in range(8):
                         mm(banks[bk][:, :], mnT[:, kc, mc * 128:(mc + 1) * 128], w[:, kc, :], kc == 0, kc == 7,
                            [B_w, B_mnT], [B_bank[bk]])
                     cp(ACT if mc == 0 else DVE, mv_sb[:, mc, (g - 2) * 512:(g - 1) * 512], banks[bk][:, :],
                        [B_bank[bk]], [B_mv])
         dma(SP, mkT_s.rearrange("p (c m) -> p c m", c=8), mk_sb, [B_mk], [B_mks])
         dma(SP, mv_s.rearrange("p (c m) -> p c m", c=2), mv_sb, [B_mv], [B_mvs])
         memset(DVE, kbarT[:], 0.0, [B_kbar])
         memset(DVE, uhalo[:], 0.0, [B_uh])
         chk(1)
         prep_slice(8)

         for qi in range(SEQ // TILE):
             tg = bi * (SEQ // TILE) + qi
             t0 = bi * SEQ + qi * TILE
             p0 = qi * TILE

             P.barrier()
             arena.off = 0
             ws = WStream(3, 8, 512)
             wco = arena.alloc([4, D], BF16)
             wao = arena.alloc([4, D], BF16)
             wmr = arena.alloc([8, D], BF16)
             B_wco, B_wao, B_wmr = Buf("wco"), Buf("wao"), Buf("wmr")
             aT = arena.alloc([8, TILE], BF16)
             B_aT = Buf("aT")
             sq = [arena.alloc([TILE], BF16) for _ in range(2)]
             B_sq = [Buf("sq0"), Buf("sq1")]
             rs1 = arena.alloc([TILE], F32)
             rs2 = arena.alloc([TILE], F32)
             B_rs = [Buf("rs1"), Buf("rs2")]
             cosb = arena.alloc([TILE], F32)
             sinb = arena.alloc([TILE], F32)
             B_cs = Buf("cossin")
             pastb = arena.alloc([4, 64], F32)
             negb = arena.alloc([4, 64], F32)
             ownb = arena.alloc([4, 64], F32)
             B_msk = Buf("masks")
             QT = arena.alloc([4, TILE], BF16)
             B_QT = Buf("QT")
             t1 = [arena.alloc([TILE], F32) for _ in range(2)]
             t2 = [arena.alloc([TILE], F32) for _ in range(2)]
             B_t1 = [Buf("t1a"), Buf("t1b")]
             B_t2 = [Buf("t2a"), Buf("t2b")]
             kb32 = arena.alloc([4, 2], F32)
             B_kb32 = Buf("kb32")
             gate_sb = arena.alloc([64], F32)
             gm = arena.alloc([64], F32)
             thr8 = arena.alloc([8, 8], F32)
             sel = arena.alloc([64], F32)
             biasb = arena.alloc([4, 64], BF16)
             B_gate, B_gm, B_thr, B_sel, B_biasb = Buf("gate"), Buf("gm"), Buf("thr"), Buf("sel"), Buf("biasb")
             biasT = [arena.alloc([TILE], BF16) for _ in range(2)]
             B_biasT = [Buf("biasT0"), Buf("biasT1")]
             PT = [arena.alloc([TILE], BF16) for _ in range(4)]
             B_PT = [Buf("PT%d" % i) for i in range(4)]
             rinv = [arena.alloc([4], F32) for _ in range(2)]
             B_rinv = [Buf("rinv0"), Buf("rinv1")]
             o_tm = arena.alloc([4, 512], BF16)
             B_otm = Buf("o_tm")
             oT = arena.alloc([4, TILE], BF16)
             B_oT = Buf("oT")
             xs = [arena.alloc([TILE], F32) for _ in range(2)]
             B_xs = [Buf("xs0"), Buf("xs1")]
             cv = [arena.alloc([TILE], F32) for _ in range(2)]
             B_cv = [Buf("cv0"), Buf("cv1")]
             yc = arena.alloc([4, TILE], BF16)
             B_yc = Buf("yc")
             merged = arena.alloc([8, TILE], BF16)
             B_mrg = Buf("merged")
             sgblk = arena.alloc([4, TILE], F32)
             sg = [sgblk[:, i, :] for i in range(4)]
             B_sg = [Buf("sg%d" % i) for i in range(4)]
             uconv = arena.alloc([4, TILE + 2], F32)
             B_u = Buf("uconv")
             cp(POOL, uconv[:, :, 0:2], uhalo[:], [B_uh], [B_u])
             xtm = [sgblk[:, 2 * i:2 * i + 2, :].rearrange("p a b -> p (a b)") for i in range(2)]
             B_xtm = [Buf("xtm0"), Buf("xtm1")]

             dma(SP, cosb, cos_d[:, p0:p0 + TILE], [], [B_cs])
             dma(SP, sinb, sin_d[:, p0:p0 + TILE], [], [B_cs])
             dma(SP, pastb, past_d[p0:p0 + TILE, :].rearrange("(s p) n -> p s n", p=128), [], [B_msk])
             dma(SP, negb, neg_d[p0:p0 + TILE, :].rearrange("(s p) n -> p s n", p=128), [], [B_msk])
             dma(SP, ownb, own_d[p0:p0 + TILE, :].rearrange("(s p) n -> p s n", p=128), [], [B_msk])
             pre_k = [ws.load(win_s[:, 2048:2560], 8), ws.load(win_s[:, 5632:6144], 8)]
             for sub in range(4):
                 s = sub % 2
                 dma(SP, xtm[s], x_d[t0 + sub * 128:t0 + (sub + 1) * 128, :], [], [B_xtm[s]])
                 for half in range(2):
                     bk = (sub * 2 + half) % 4
                     for j in range(4):
                         kc = half * 4 + j
                         tr(banks[bk][:, j * 128:(j + 1) * 128], xtm[s][:, kc * 128:(kc + 1) * 128], ident32[:],
                            [B_xtm[s], B_const], [B_bank[bk]])
                     cp(ACT if half == 0 else DVE, hT[:, half * 4:half * 4 + 4, sub * 128:(sub + 1) * 128],
                        banks[bk][:].rearrange("p (a b) -> p a b", a=4), [B_bank[bk]], [B_hT])


             norm_fm(hT, B_hT, TILE, 0, aT, B_aT, sq, B_sq, rs1, rs2, B_rs, 0)

             chk(2.5)
             prep_slice(6)
             def rope_pair(col_a, col_b, dst, B_dst, dst_off, pre=None):
                 if pre is not None:
                     (wa, B_wa), (wb, B_wb) = pre
                 else:
                     wa, B_wa = ws.load(win_s[:, col_a:col_a + 512], 8)
                     wb, B_wb = ws.load(win_s[:, col_b:col_b + 512], 8)
                 for c in range(4):
                     s = c % 2
                     proj_fm(wa, B_wa, c * 128, aT, B_aT, TILE, 0 + s)
                     proj_fm(wb, B_wb, c * 128, aT, B_aT, TILE, 2 + s)
                     tt(DVE, t1[s], banks[0 + s][:, :], cosb, ALU.mult, [B_bank[0 + s], B_cs], [B_t1[s]])
                     tt(DVE, t2[s], banks[2 + s][:, :], sinb, ALU.mult, [B_bank[2 + s], B_cs], [B_t2[s]])
                     tt(DVE, dst[:, c, dst_off:dst_off + TILE], t1[s], t2[s], ALU.add,
                        [B_t1[s], B_t2[s]], [B_dst])

             rope_pair(2048, 5632, KT, B_KT, p0, pre=pre_k)
             prep_slice(6)
             wv, B_wv = ws.load(win_s[:, 2560:3072], 8)
             for sub in range(4):
                 bk = sub % 2
                 for kc in range(8):
                     mm(banks[bk][:, :], aT[:, kc, sub * 128:(sub + 1) * 128], wv[:, kc, :], kc == 0, kc == 7,
                        [B_wv, B_aT], [B_bank[bk]])
                 cp(ACT if sub % 2 == 0 else DVE, V[:, qi * 4 + sub, :, 0:64],
                    banks[bk][:].rearrange("p (h d) -> p h d", h=8), [B_bank[bk]], [B_V])
             rope_pair(1536, 5120, QT, B_QT, 0)
             prep_slice(6)
             dma(POOL, wco, wco_s.rearrange("(kc p) n -> p kc n", p=128), [], [B_wco])
             dma(POOL, wao, wao_s.rearrange("(kc p) n -> p kc n", p=128), [], [B_wao])
             dma(POOL, wmr, wm_s.rearrange("(kc p) n -> p kc n", p=128), [], [B_wmr])

             chk(3)
             for c in range(4):
                 P.op(DVE, (lambda o_, i_: lambda e: e.tensor_reduce(o_, i_, AX.X, ALU.add))(
                     kb32[:, c, :], KT[:, c, p0:p0 + TILE].rearrange("p (b k) -> p b k", b=2)),
                     reads=[B_KT], writes=[B_kb32])
             ts(DVE, kbarT[:, :, 2 * qi:2 * qi + 2], kb32, 1.0 / 256.0, None, ALU.mult, None, [B_kb32], [B_kbar])

             for sub in (range(4) if qi >= 2 else ()):
                 for h in range(8):
                     pb = (h % 2) * 64
                     mm(banks[4][:, h * 8:(h + 1) * 8], QT[pb:pb + 64, h // 2, sub * 128:(sub + 1) * 128],
                        kbarT[pb:pb + 64, h // 2, :], True, True, [B_QT, B_kbar], [B_bank[4]])
                 cp(ACT, gate_sb, banks[4][:, 0:64], [B_bank[4]], [B_gate])
                 tt(DVE, gm, gate_sb, pastb[:, sub, :], ALU.mult, [B_gate, B_msk], [B_gm])
                 tt(DVE, gm, gm, negb[:, sub, :], ALU.add, [B_gm, B_msk], [B_gm])
                 for h in range(8):
                     P.op(DVE, (lambda h: lambda e: e.max(out=thr8[:, h, :], in_=gm[:, h * 8:(h + 1) * 8]))(h),
                          reads=[B_gm], writes=[B_thr])
                 for h in range(8):
                     ts(DVE, sel[:, h * 8:(h + 1) * 8], gm[:, h * 8:(h + 1) * 8], thr8[:, h, 2:3], None,
                        ALU.is_ge, None, [B_gm, B_thr], [B_sel])
                 tt(DVE, sel, sel, pastb[:, sub, :], ALU.mult, [B_sel, B_msk], [B_sel])
                 tt(DVE, sel, sel, ownb[:, sub, :], ALU.add, [B_sel, B_msk], [B_sel])
                 ts(DVE, biasb[:, sub, :], sel, -1.0, NEGB, ALU.add, ALU.mult, [B_sel], [B_biasb])
                 if dbg:
                     dma(SP, dbg_d["bias"][t0 + sub * 128:t0 + (sub + 1) * 128, :], biasb[:, sub, :], [B_biasb], [])
                     dma(SP, dbg_d["gate"][t0 + sub * 128:t0 + (sub + 1) * 128, :], gm, [B_gm], [])

             chk(3.3)
             prep_slice(4)
             nkt = 4 * (qi + 1)
             def attend(h):
                 pb = (h % 2) * 64
                 hs = h % 2
                 pvb = bank_bf(4 + hs)
                 if qi >= 2:
                     for sub in range(4):
                         tr(pvb[0:8, sub * 128:(sub + 1) * 128], biasb[:, sub, h * 8:(h + 1) * 8], identb[:],
                            [B_biasb, B_const], [B_bank[4 + hs]])
                     cp(DVE, biasT[hs][0:8, :], pvb[0:8, 0:TILE], [B_bank[4 + hs]], [B_biasT[hs]])
                 ob = 6 + hs
                 O3 = banks[ob][:, 0:260].rearrange("p (j d) -> p j d", j=4)

                 def s_stage(kt):
                     sb_ = 2 * hs + kt % 2
                     c0 = max(0, kt - 4 * qi) * 128
                     need_bias = qi >= 2 and kt < 4 * qi + 2
                     mm(banks[sb_][:, c0:TILE], KT[pb:pb + 64, h // 2, kt * 128:(kt + 1) * 128],
                        QT[pb:pb + 64, h // 2, c0:TILE], True, not need_bias, [B_KT, B_QT], [B_bank[sb_]])
                     if need_bias:
                         mm(banks[sb_][:, c0:TILE], ebb[0:8, kt // 2, :], biasT[hs][0:8, c0:TILE], False, True,
                            [B_const, B_biasT[hs]], [B_bank[sb_]])
                     pt = 2 * hs + kt % 2
                     act(PT[pt][:, c0:TILE], banks[sb_][:, c0:TILE], AF.Exp, [B_bank[sb_]], [B_PT[pt]], scale=0.125)
                     if kt >= 4 * qi:
                         tt(DVE, PT[pt][:, c0:c0 + 128], PT[pt][:, c0:c0 + 128], trib[:], ALU.mult,
                            [B_PT[pt], B_const], [B_PT[pt]])

                 def pv_stage(kt):
                     pt = 2 * hs + kt % 2
                     j0 = max(0, kt - 4 * qi)
                     for j in range(j0, 4):
                         first = (kt == 0 and j == j0)
                         last = (kt == nkt - 1 and j == 3)
                         mm(O3[:, j, :], PT[pt][:, j * 128:(j + 1) * 128], V[:, kt, h, :], first, last,
                            [B_PT[pt], B_V], [B_bank[ob]], skip=True)

                 s_stage(0)
                 yield
                 for kt in range(nkt):
                     if kt + 1 < nkt:
                         s_stage(kt + 1)
                         yield
                     pv_stage(kt)
                     yield
                 P.op(DVE, (lambda O3=O3, hs=hs: lambda e: e.reciprocal(rinv[hs], O3[:, :, 64]))(),
                      reads=[B_bank[ob]], writes=[B_rinv[hs]])
                 for j in range(4):
                     if j % 2 == 0:
                         ts(DVE, o_tm[:, j, h * 64:(h + 1) * 64], O3[:, j, 0:64], rinv[hs][:, j:j + 1], None,
                            ALU.mult, None, [B_bank[ob], B_rinv[hs]], [B_otm])
                     else:
                         act(o_tm[:, j, h * 64:(h + 1) * 64], O3[:, j, 0:64], AF.Copy,
                             [B_bank[ob], B_rinv[hs]], [B_otm], scale=rinv[hs][:, j:j + 1])

             for hp in range(4):
                 prep_slice(8)
                 gens = [attend(2 * hp), attend(2 * hp + 1)]
                 while gens:
                     for g_ in list(gens):
                         try:
                             next(g_)
                         except StopIteration:
                             gens.remove(g_)
             if dbg:
                 dma(SP, dbg_d["o"][t0:t0 + TILE, :].rearrange("(j p) n -> p j n", p=128), o_tm, [B_otm], [])
             for j in range(4):
                 bk = 4 + j % 2
                 pvb = bank_bf(bk)
                 for c in range(4):
                     tr(pvb[:, c * 128:(c + 1) * 128], o_tm[:, j, c * 128:(c + 1) * 128], identb[:],
                        [B_otm, B_const], [B_bank[bk]])
                 cp(ACT if j % 2 == 0 else DVE, oT[:, :, j * 128:(j + 1) * 128],
                    pvb[:, 0:512].rearrange("p (a b) -> p a b", a=4), [B_bank[bk]], [B_oT])

             chk(3.6)
             prep_slice(6)
             wx, B_wx = ws.load(win_s[:, 0:512], 8)
             wc, B_wc = ws.load(win_s[:, 1024:1536], 8)
             wbg, B_wbg = ws.load(win_s[:, 512:1024], 8)
             for c in range(4):
                 s = c % 2
                 proj_fm(wx, B_wx, c * 128, aT, B_aT, TILE, 0 + s)
                 proj_fm(wc, B_wc, c * 128, aT, B_aT, TILE, 2 + s)
                 proj_fm(wbg, B_wbg, c * 128, aT, B_aT, TILE, 4 + s)
                 cp(ACT, xs[s], banks[0 + s][:, :], [B_bank[0 + s]], [B_xs[s]])
                 tt(DVE, uconv[:, c, 2:TILE + 2], banks[2 + s][:, :], xs[s], ALU.mult, [B_bank[2 + s], B_xs[s]], [B_u])
                 ts(DVE, cv[s], uconv[:, c, 2:TILE + 2], wconv[:, c * 3 + 2:c * 3 + 3], None, ALU.mult, None,
                    [B_u, B_const], [B_cv[s]])
                 stt(DVE, cv[s], uconv[:, c, 1:TILE + 1], wconv[:, c * 3 + 1:c * 3 + 2], cv[s], ALU.mult, ALU.add,
                     [B_u, B_cv[s], B_const], [B_cv[s]])
                 stt(DVE, cv[s], uconv[:, c, 0:TILE], wconv[:, c * 3:c * 3 + 1], cv[s], ALU.mult, ALU.add,
                     [B_u, B_cv[s], B_const], [B_cv[s]])
                 tt(DVE, yc[:, c, :], banks[4 + s][:, :], cv[s], ALU.mult, [B_bank[4 + s], B_cv[s]], [B_yc])
             cp(POOL, uhalo[:], uconv[:, :, TILE:TILE + 2], [B_u], [B_uh])

             wgs = {}
             for oc in range(8):
                 if oc % 4 == 0:
                     gci = 3072 + (oc // 4) * 512
                     gai = 4096 + (oc // 4) * 512
                     wgs["c"] = ws.load(win_s[:, gci:gci + 512], 8)
                     wgs["a"] = ws.load(win_s[:, gai:gai + 512], 8)
                 wgc, B_wgc = wgs["c"]
                 wga, B_wga = wgs["a"]
                 gb = 4 * (oc % 2)
                 proj_fm(wgc, B_wgc, (oc % 4) * 128, aT, B_aT, TILE, gb + 0)
                 proj_fm(wga, B_wga, (oc % 4) * 128, aT, B_aT, TILE, gb + 1)
                 for cc in range(4):
                     mm(banks[gb + 2][:, :], wco[:, cc, oc * 128:(oc + 1) * 128], yc[:, cc, :], cc == 0, cc == 3,
                        [B_wco, B_yc], [B_bank[gb + 2]])
                 for cc in range(4):
                     mm(banks[gb + 3][:, :], wao[:, cc, oc * 128:(oc + 1) * 128], oT[:, cc, :], cc == 0, cc == 3,
                        [B_wao, B_oT], [B_bank[gb + 3]])
                 act(sg[0], banks[gb + 0][:, :], AF.Sigmoid, [B_bank[gb + 0]], [B_sg[0]])
                 act(sg[1], banks[gb + 1][:, :], AF.Sigmoid, [B_bank[gb + 1]], [B_sg[1]])
                 tt(DVE, sg[2], banks[gb + 2][:, :], sg[0], ALU.mult, [B_bank[gb + 2], B_sg[0]], [B_sg[2]])
                 tt(DVE, sg[3], banks[gb + 3][:, :], sg[1], ALU.mult, [B_bank[gb + 3], B_sg[1]], [B_sg[3]])
                 tt(DVE, merged[:, oc, :], sg[2], sg[3], ALU.add, [B_sg[2], B_sg[3]], [B_mrg])
             for oc in range(8):
                 bk = 4 + oc % 2
                 for kc in range(8):
                     mm(banks[bk][:, :], wmr[:, kc, oc * 128:(oc + 1) * 128], merged[:, kc, :], kc == 0, kc == 7,
                        [B_wmr, B_mrg], [B_bank[bk]])
                 tt(DVE, hT[:, oc, :], banks[bk][:, :], hT[:, oc, :], ALU.add, [B_bank[bk], B_hT], [B_hT])
             if dbg:
                 dma(SP, dbg_d["h1"][tg].rearrange("p (a b) -> p a b", a=8), hT[:], [B_hT], [])

             chk(4)
             prep_slice(10)
             P.barrier()
             arena.off = 0
             ws = WStream(3, 8, 512)
             aT = arena.alloc([8, TILE], BF16)
             B_aT = Buf("aT")
             sq = [arena.alloc([TILE], BF16) for _ in range(2)]
             B_sq = [Buf("sq0"), Buf("sq1")]
             rs1 = arena.alloc([TILE], F32)
             rs2 = arena.alloc([TILE], F32)
             B_rs = [Buf("rs1"), Buf("rs2")]
             mk_sb = arena.alloc([8, NMEM], BF16)
             mv_sb = arena.alloc([2, D], BF16)
             B_mk, B_mv = Buf("mk"), Buf("mv")
             qxT = arena.alloc([8, TILE], BF16)
             B_qx = Buf("qxT")
             PX = [arena.alloc([TILE], BF16) for _ in range(4)]
             B_PX = [Buf("PX%d" % i) for i in range(4)]
             rxi = [arena.alloc([TILE], F32) for _ in range(2)]
             B_rxi = [Buf("rxi0"), Buf("rxi1")]
             oxT = arena.alloc([8, TILE], BF16)
             B_ox = Buf("oxT")
             dma(SP, mk_sb, mkT_s.rearrange("p (c m) -> p c m", c=8), [B_mks], [B_mk])
             dma(SP, mv_sb, mv_s.rearrange("p (c m) -> p c m", c=2), [B_mvs], [B_mv])
             norm_fm(hT, B_hT, TILE, 8, aT, B_aT, sq, B_sq, rs1, rs2, B_rs, 0)
             for g in range(2):
                 w, B_w = ws.load(wxq_s[:, g * 512:(g + 1) * 512], 8)
                 for cc in range(4):
                     c = g * 4 + cc
                     bk = c % 2
                     proj_fm(w, B_w, cc * 128, aT, B_aT, TILE, bk)
                     cp(ACT if c % 2 == 0 else DVE, qxT[:, c, :], banks[bk][:, :], [B_bank[bk]], [B_qx])
             for h in range(4):
                 hs = h % 2
                 for mc in range(2):
                     bk = 2 + mc
                     for dc in range(2):
                         mm(banks[bk][:, :], mk_sb[:, h * 2 + dc, mc * 128:(mc + 1) * 128], qxT[:, h * 2 + dc, :],
                            dc == 0, dc == 1, [B_mk, B_qx], [B_bank[bk]])
                     act(PX[hs * 2 + mc], banks[bk][:, :], AF.Exp, [B_bank[bk]], [B_PX[hs * 2 + mc]], scale=1.0 / 16.0)
                 for mc in range(2):
                     mm(banks[4][:, :], onesb[:], PX[hs * 2 + mc], mc == 0, mc == 1,
                        [B_const, B_PX[hs * 2 + mc]], [B_bank[4]])
                 P.op(DVE, (lambda hs=hs: lambda e: e.reciprocal(rxi[hs], banks[4][:, :]))(),
                      reads=[B_bank[4]], writes=[B_rxi[hs]])
                 for dc in range(2):
                     bk = 5 + dc
                     for mc in range(2):
                         c0 = h * 256 + dc * 128
                         mm(banks[bk][:, :], mv_sb[:, mc, c0:c0 + 128], PX[hs * 2 + mc], mc == 0, mc == 1,
                            [B_mv, B_PX[hs * 2 + mc]], [B_bank[bk]])
                     tt(DVE, oxT[:, h * 2 + dc, :], banks[bk][:, :], rxi[hs], ALU.mult,
                        [B_bank[bk], B_rxi[hs]], [B_ox])
             for g in range(2):
                 w, B_w = ws.load(wxo_s[:, g * 512:(g + 1) * 512], 8)
                 for cc in range(4):
                     oc = g * 4 + cc
                     bk = oc % 2
                     proj_fm(w, B_w, cc * 128, oxT, B_ox, TILE, bk)
                     tt(DVE, hT[:, oc, :], banks[bk][:, :], hT[:, oc, :], ALU.add, [B_bank[bk], B_hT], [B_hT])
             if dbg:
                 dma(SP, dbg_d["h2"][tg].rearrange("p (a b) -> p a b", a=8), hT[:], [B_hT], [])

             chk(5)
             prep_slice(10)
             TC = 256
             for cs in range(0 if skip_c else TILE // TC):
                 c_t0 = cs * TC
                 P.barrier()
                 arena.off = 0
                 aT = arena.alloc([8, TC], BF16)
                 B_aT = Buf("aT")
                 IT = arena.alloc([TC], BF16)
                 JT = arena.alloc([TC], BF16)
                 gT = arena.alloc([TC], BF16)
                 B_IT, B_JT, B_gT = Buf("IT"), Buf("JT"), Buf("gT")
                 iorep = arena.alloc([128, 16], BF16)
                 B_iorep = Buf("iorep")
                 dma(POOL, iorep, iorep_d.rearrange("p (i t) -> p i t", t=16), [], [B_iorep])
                 mark = arena.off
                 ws = WStream(3, 8, 512)
                 sq = [arena.alloc([TILE], BF16) for _ in range(2)]
                 B_sq = [Buf("sq0"), Buf("sq1")]
                 rs1 = arena.alloc([TILE], F32)
                 rs2 = arena.alloc([TILE], F32)
                 B_rs = [Buf("rs1"), Buf("rs2")]
                 pqT = arena.alloc([16, TC], BF16)
                 B_pq = Buf("pqT")
                 class _NS:
                     pass

                 subs = []
                 for _si in range(TC // 128):
                     W = _NS()
                     W.sc = arena.alloc([2048], F32)
                     W.B_sc = Buf("sc")
                     W.scrs = [arena.alloc([128], F32) for _ in range(16)]
                     W.B_scrs = [Buf("scr%d" % i) for i in range(16)]
                     W.B_Va = [Buf("Va%d" % i) for i in range(16)]
                     W.B_Vb = [Buf("Vb%d" % i) for i in range(16)]
                     W.B_Ia = [Buf("Ia%d" % i) for i in range(16)]
                     W.B_Ib = [Buf("Ib%d" % i) for i in range(16)]
                     W.V16 = arena.alloc([16, 16], F32)
                     W.I16 = arena.alloc([16, 16], U32)
                     W.I16f = arena.alloc([16, 16], F32)
                     W.B_I16f = Buf("I16f")
                     W.cand = arena.alloc([8, 16, 16], F32)
                     W.B_cand = Buf("cand")
                     W.cscrs = [arena.alloc([256], F32) for _ in range(8)]
                     W.B_cscrs = [Buf("cscr%d" % i) for i in range(8)]
                     W.B_Ta = [Buf("Ta%d" % i) for i in range(8)]
                     W.B_Tb = [Buf("Tb%d" % i) for i in range(8)]
                     W.B_Pa = [Buf("Pa%d" % i) for i in range(8)]
                     W.B_Pb = [Buf("Pb%d" % i) for i in range(8)]
                     W.T16 = arena.alloc([8, 16], F32)
                     W.P16 = arena.alloc([8, 16], U32)
                     W.K1 = arena.alloc([8, 16], U32)
                     W.K2 = arena.alloc([8, 16], U32)
                     W.K1f = arena.alloc([8, 16], F32)
                     W.K2f = arena.alloc([8, 16], F32)
                     W.B_K = Buf("K12")
                     W.eq = W.cand
                     W.B_eq = Buf("eq")
                     W.nmx = arena.alloc([8], F32)
                     W.zs = arena.alloc([8], F32)
                     W.zr = arena.alloc([8], F32)
                     W.B_nmx, W.B_zs = Buf("nmx"), Buf("zs")
                     W.Itm = arena.alloc([128], F32)
                     W.Jtm = arena.alloc([128], F32)
                     W.gtm = arena.alloc([128], F32)
                     W.B_Itm, W.B_Jtm, W.B_gtm = Buf("Itm"), Buf("Jtm"), Buf("gtm")
                     subs.append(W)

                 norm_fm(hT[:, :, c_t0:c_t0 + TC], B_hT, TC, 24, aT, B_aT, sq, B_sq, rs1, rs2, B_rs, 0)
                 for g in range(4):
                     w, B_w = ws.load(wpq_s[:, g * 512:(g + 1) * 512], 8)
                     for cc in range(4):
                         c = g * 4 + cc
                         bk = 4 + c % 2
                         proj_fm(w, B_w, cc * 128, aT, B_aT, TC, bk)
                         cp(ACT if c % 2 == 0 else DVE, pqT[:, c, :], banks[bk][:, :TC], [B_bank[bk]], [B_pq])
                 chk(5.1)
                 prep_slice(8)
                 def mk_max(o_, i_):
                     return lambda e: e.max(out=o_, in_=i_)

                 def mk_idx(o_, m_, v_):
                     return lambda e: e.max_index(out=o_, in_max=m_, in_values=v_)

                 def mk_rep(o_, m_, v_):
                     return lambda e: e.match_replace(out=o_, in_to_replace=m_, in_values=v_, imm_value=-1e30)

                 def mk_ss(o_, i_, v_, op_):
                     return lambda e: e.tensor_single_scalar(o_, i_, v_, op_)

                 def mk_rcp(o_, i_):
                     return lambda e: e.reciprocal(o_, i_)

                 def mk_red(o_, i_):
                     return lambda e: e.tensor_reduce(o_, i_, AX.X, ALU.add)

                 def topk_sub(tsub, W):
                     b0 = 4 * tsub
                     sc, V16, I16, I16f, cand, T16, P16 = W.sc, W.V16, W.I16, W.I16f, W.cand, W.T16, W.P16
                     for c in range(16):
                         bk = b0 + c // 4
                         mm(banks[bk][:, (c % 4) * 128:(c % 4 + 1) * 128], pqT[:, c, tsub * 128:(tsub + 1) * 128],
                            skT[:, c, :], True, True, [B_pq, B_skT], [B_bank[bk]])
                     for q4 in range(4):
                         cp(ACT if q4 % 2 == 0 else DVE, sc[:, q4 * 512:(q4 + 1) * 512], banks[b0 + q4][:, :],
                            [B_bank[b0 + q4]], [W.B_sc])
                     yield
                     for c in range(16):
                         P.op(DVE, mk_max(V16[:, c, 0:8], sc[:, c * 128:(c + 1) * 128]), reads=[W.B_sc],
                              writes=[W.B_Va[c]])
                     yield
                     for c in range(16):
                         scc = sc[:, c * 128:(c + 1) * 128]
                         P.op(DVE, mk_idx(I16[:, c, 0:8], V16[:, c, 0:8], scc), reads=[W.B_sc, W.B_Va[c]],
                              writes=[W.B_Ia[c]])
                         P.op(DVE, mk_rep(W.scrs[c], V16[:, c, 0:8], scc), reads=[W.B_sc, W.B_Va[c]],
                              writes=[W.B_scrs[c]])
                     yield
                     for c in range(16):
                         P.op(DVE, mk_max(V16[:, c, 8:16], W.scrs[c]), reads=[W.B_scrs[c]], writes=[W.B_Vb[c]])
                     yield
                     for c in range(16):
                         P.op(DVE, mk_idx(I16[:, c, 8:16], V16[:, c, 8:16], W.scrs[c]),
                              reads=[W.B_scrs[c], W.B_Vb[c]], writes=[W.B_Ib[c]])
                     B_V16 = W.B_Va + W.B_Vb
                     B_I16 = W.B_Ia + W.B_Ib
                     cp(DVE, I16f, I16, B_I16, [W.B_I16f])
                     V4 = V16.rearrange("p (h two) k -> p h two k", two=2)
                     I4 = I16f.rearrange("p (h two) k -> p h two k", two=2)
                     tt(DVE, cand, V4[:, :, 0, :].unsqueeze(3).to_broadcast([128, 8, 16, 16]),
                        V4[:, :, 1, :].unsqueeze(2).to_broadcast([128, 8, 16, 16]), ALU.add, B_V16, [W.B_cand])
                     yield
                     chs = [cand[:, h, :, :].rearrange("p a b -> p (a b)") for h in range(8)]
                     for h in range(8):
                         P.op(DVE, mk_max(T16[:, h, 0:8], chs[h]), reads=[W.B_cand], writes=[W.B_Ta[h]])
                     yield
                     for h in range(8):
                         P.op(DVE, mk_idx(P16[:, h, 0:8], T16[:, h, 0:8], chs[h]), reads=[W.B_cand, W.B_Ta[h]],
                              writes=[W.B_Pa[h]])
                         P.op(DVE, mk_rep(W.cscrs[h], T16[:, h, 0:8], chs[h]), reads=[W.B_cand, W.B_Ta[h]],
                              writes=[W.B_cscrs[h]])
                     yield
                     for h in range(8):
                         P.op(DVE, mk_max(T16[:, h, 8:16], W.cscrs[h]), reads=[W.B_cscrs[h]], writes=[W.B_Tb[h]])
                     yield
                     for h in range(8):
                         P.op(DVE, mk_idx(P16[:, h, 8:16], T16[:, h, 8:16], W.cscrs[h]),
                              reads=[W.B_cscrs[h], W.B_Tb[h]], writes=[W.B_Pb[h]])
                     B_T16l = W.B_Ta + W.B_Tb
                     B_P16l = W.B_Pa + W.B_Pb
                     ts(DVE, W.nmx, T16[:, :, 0], -1.0, None, ALU.mult, None, B_T16l, [W.B_nmx])
                     yield
                     for h in range(8):
                         act(W.gtm[:, h * 16:(h + 1) * 16], T16[:, h, :], AF.Exp, B_T16l + [W.B_nmx],
                             [W.B_gtm, W.B_zs], bias=W.nmx[:, h:h + 1], scale=1.0, accum=W.zs[:, h:h + 1])
                     P.op(DVE, mk_ss(W.K1, P16, 4, ALU.logical_shift_right), reads=B_P16l, writes=[W.B_K])
                     P.op(DVE, mk_ss(W.K2, P16, 15, ALU.bitwise_and), reads=B_P16l, writes=[W.B_K])
                     cp(DVE, W.K1f, W.K1, [W.B_K], [W.B_K])
                     cp(DVE, W.K2f, W.K2, [W.B_K], [W.B_K])
                     yield
                     P.op(DVE, mk_rcp(W.zr, W.zs), reads=[W.B_zs], writes=[W.B_nmx])
                     g3v = W.gtm.rearrange("p (h k) -> p h k", h=8)
                     tt(DVE, g3v, g3v, W.zr.unsqueeze(2).to_broadcast([128, 8, 16]), ALU.mult,
                        [W.B_gtm, W.B_nmx], [W.B_gtm])
                     yield
                     io4 = iota16[:].unsqueeze(1).unsqueeze(1).to_broadcast([128, 8, 16, 16])
                     for (Kf, half, dst, B_dst) in ((W.K1f, 0, W.Itm, W.B_Itm), (W.K2f, 1, W.Jtm, W.B_Jtm)):
                         tt(DVE, W.eq, Kf.unsqueeze(3).to_broadcast([128, 8, 16, 16]), io4, ALU.is_equal,
                            [W.B_K, B_const, W.B_cand], [W.B_eq])
                         yield
                         tt(DVE, W.eq, W.eq, I4[:, :, half, :].unsqueeze(2).to_broadcast([128, 8, 16, 16]),
                            ALU.mult, [W.B_eq, W.B_I16f], [W.B_eq])
                         yield
                         P.op(DVE, mk_red(dst.rearrange("p (h k) -> p h k", h=8), W.eq), reads=[W.B_eq],
                              writes=[B_dst])
                         yield
                     tb = b0
                     for (src, B_src, col) in ((W.Itm, W.B_Itm, 0), (W.Jtm, W.B_Jtm, 128), (W.gtm, W.B_gtm, 256)):
                         tr(banks[tb][:, col:col + 128], src, ident32[:], [B_src, B_const], [B_bank[tb]])
                     cp(ACT, IT[:, tsub * 128:(tsub + 1) * 128], banks[tb][:, 0:128], [B_bank[tb]], [B_IT])
                     cp(ACT, JT[:, tsub * 128:(tsub + 1) * 128], banks[tb][:, 128:256], [B_bank[tb]], [B_JT])
                     cp(ACT, gT[:, tsub * 128:(tsub + 1) * 128], banks[tb][:, 256:384], [B_bank[tb]], [B_gT])

                 prep_slice(16)
                 gens = [topk_sub(i, subs[i]) for i in range(TC // 128)]
                 while gens:
                     for g_ in list(gens):
                         try:
                             next(g_)
                         except StopIteration:
                             gens.remove(g_)

                 chk(6)
                 prep_slice(128)
                 P.barrier()
                 arena.off = mark
                 G = arena.alloc([128, TC], BF16)
                 B_G = Buf("G")
                 NI = 4
                 NBUF = 2
                 ubuf = [arena.alloc([8, NI * 128], BF16) for _ in range(NBUF)]
                 vbuf = [arena.alloc([NI, D], BF16) for _ in range(NBUF)]
                 B_ubuf = [Buf("ub%d" % i) for i in range(NBUF)]
                 B_vbuf = [Buf("vb%d" % i) for i in range(NBUF)]
                 actb = [arena.alloc([TC], BF16) for _ in range(4)]
                 gab = [arena.alloc([TC], BF16) for _ in range(4)]
                 B_actb = [Buf("act%d" % i) for i in range(4)]
                 B_gab = [Buf("ga%d" % i) for i in range(4)]
                 TB = 16
                 oi_off = arena.off
                 OI = [arena.alloc([128, TB], BF16) for _ in range(2)]
                 OJ = [arena.alloc([128, TB], BF16) for _ in range(2)]
                 B_OI = [Buf("OI%d" % i) for i in range(2)]
                 B_OJ = [Buf("OJ%d" % i) for i in range(2)]
                 for b in range(TC // TB):
                     s = b % 2
                     tsl = slice(b * TB, (b + 1) * TB)
                     tt(DVE, OI[s], IT[:, tsl].unsqueeze(1).to_broadcast([128, 128, TB]), iorep, ALU.is_equal,
                        [B_IT, B_iorep], [B_OI[s]])
                     tt(DVE, OJ[s], JT[:, tsl].unsqueeze(1).to_broadcast([128, 128, TB]), iorep, ALU.is_equal,
                        [B_JT, B_iorep], [B_OJ[s]])
                     tt(DVE, OI[s], OI[s], gT[:, tsl].unsqueeze(1).to_broadcast([128, 128, TB]), ALU.mult,
                        [B_OI[s], B_gT], [B_OI[s]])
                     for tl in range(TB):
                         t = b * TB + tl
                         bk = 4 + (t // 4) % 2
                         g4v = banks[bk][:].rearrange("p (i t) -> p i t", t=4)
                         mm(g4v[:, :, t % 4], OJ[s][:, :, tl], OI[s][:, :, tl], True, True, [B_OI[s], B_OJ[s]],
                            [B_bank[bk]])
                         if t % 4 == 3:
                             cp(ACT, G[:, :, t - 3:t + 1], g4v, [B_bank[bk]], [B_G])
                 chk(7)
                 LA = 3

                 def u_stage(i):
                     gi, ii = divmod(i, NI)
                     s = gi % NBUF
                     if ii == 0:
                         dma(SP, ubuf[s], uT_s[:, :, gi * NI * 128:(gi + 1) * NI * 128], [B_uTs], [B_ubuf[s]])
                         dma(SP, vbuf[s],
                             v_s[gi * NI * 128:(gi + 1) * NI * 128, :].rearrange("(i p) d -> p i d", p=128),
                             [B_vs], [B_vbuf[s]])
                     bk = 4 + i % 4
                     for kc in range(8):
                         mm(banks[bk][:, :TC], ubuf[s][:, kc, ii * 128:(ii + 1) * 128], aT[:, kc, :],
                            kc == 0, kc == 7, [B_ubuf[s], B_aT], [B_bank[bk]])
                     r4 = i % 4
                     act(actb[r4], banks[bk][:, :TC], AF.Gelu_apprx_tanh, [B_bank[bk]], [B_actb[r4]])
                     tt(DVE, gab[r4], actb[r4], G[:, i, :], ALU.mult, [B_actb[r4], B_G], [B_gab[r4]])

                 def v_stage(i):
                     gi, ii = divmod(i, NI)
                     s = gi % NBUF
                     r4 = i % 4
                     for tsub in range(2):
                         for dh in range(2):
                             ob = tsub * 2 + dh
                             mm(banks[ob][:, :], gab[r4][:, tsub * 128:(tsub + 1) * 128],
                                vbuf[s][:, ii, dh * 512:(dh + 1) * 512], i == 0, i == 127,
                                [B_gab[r4], B_vbuf[s]], [B_bank[ob]])

                 for i in range(LA):
                     u_stage(i)
                 for i in range(128):
                     if i + LA < 128:
                         u_stage(i + LA)
                     v_stage(i)
                 chk(8)
                 P.barrier()
                 arena.off = oi_off
                 pe_sb = arena.alloc([D], F32)
                 h3 = arena.alloc([D], F32)
                 junk = arena.alloc([D], F32)
                 ssq = arena.alloc([2], F32)
                 gfin = arena.alloc([D], F32)
                 osb = arena.alloc([D], F32)
                 B_pe, B_h3, B_junk, B_ssq, B_gfin, B_osb = (Buf("pe"), Buf("h3"), Buf("junk"), Buf("ssq"),
                                                            Buf("gfin"), Buf("osb"))
                 dma(SP, gfin, gfin_d, [], [B_gfin])
                 for tsub in range(2):
                     for dh in range(2):
                         cp(ACT, pe_sb[:, dh * 512:(dh + 1) * 512], banks[tsub * 2 + dh][:, :],
                            [B_bank[tsub * 2 + dh]], [B_pe])
                     if dbg:
                         r0 = t0 + c_t0 + tsub * 128
                         dma(SP, dbg_d["pe"][r0:r0 + 128, :], pe_sb, [B_pe], [])
                     for half in range(2):
                         bk = 4 + half
                         for j in range(4):
                             kc = half * 4 + j
                             tr(banks[bk][:, j * 128:(j + 1) * 128],
                                hT[:, kc, c_t0 + tsub * 128:c_t0 + (tsub + 1) * 128], ident32[:],
                                [B_hT, B_const], [B_bank[bk]])
                         tt(DVE, h3[:, half * 512:(half + 1) * 512], banks[bk][:, :],
                            pe_sb[:, half * 512:(half + 1) * 512], ALU.add, [B_bank[bk], B_pe], [B_h3])
                     act(junk, h3, AF.Square, [B_h3], [B_junk, B_ssq], accum=ssq[:, 0:1])
                     act(ssq[:, 1:2], ssq[:, 0:1], AF.Sqrt, [B_ssq], [B_ssq], bias=EPS, scale=1.0 / D)
                     P.op(DVE, lambda e: e.reciprocal(ssq[:, 0:1], ssq[:, 1:2]), reads=[B_ssq], writes=[B_ssq])
                     stt(DVE, osb, h3, ssq[:, 0:1], gfin, ALU.mult, ALU.mult, [B_h3, B_ssq, B_gfin], [B_osb])
                     r0 = t0 + c_t0 + tsub * 128
                     dma(SP, out_d[r0:r0 + 128, :], osb, [B_osb], [])

    except _Stop:
        pass
    P.emit()
    return nc


def _tables():
    half = 32
    inv = (10000.0 ** (-np.arange(half, dtype=np.float32) / half)).astype(np.float32)
    ang = np.arange(SEQ, dtype=np.float32)[None, :] * inv[:, None]
    cos, sin = np.cos(ang).astype(np.float32), np.sin(ang).astype(np.float32)
    cos64 = np.concatenate([cos, cos], 0)
    sin64 = np.concatenate([-sin, sin], 0)
    cosT = np.concatenate([cos64, cos64], 0)
    sinT = np.concatenate([sin64, sin64], 0)
    blk = np.arange(SEQ) // 256
    n = np.arange(8)
    past = (n[None, :] < blk[:, None]).astype(np.float32)
    own = (n[None, :] == blk[:, None]).astype(np.float32)
    neg = (past - 1.0) * 1e30
    rep = lambda a: np.ascontiguousarray(np.tile(a, (1, 8)).astype(np.float32))
    tri = (np.arange(128)[:, None] <= np.arange(128)[None, :]).astype(np.float32)
    eb = np.zeros((8, 8, 128), np.float32)
    for b in range(8):
        eb[b, b, :] = 1.0
    return dict(cosT=np.ascontiguousarray(cosT), sinT=np.ascontiguousarray(sinT), past64=rep(past),
                neg64=rep(neg), own64=rep(own), tri=tri, eb=eb.reshape(8, 1024),
                ident=np.eye(128, dtype=np.float32),
                iota128=np.ascontiguousarray(np.tile(np.arange(128, dtype=np.float32), (128, 1))),
                iota16=np.ascontiguousarray(np.tile(np.arange(16, dtype=np.float32), (128, 1))),
                iorep=np.ascontiguousarray(np.tile(np.repeat(np.arange(128, dtype=np.float32), 16), (128, 1))))


def _shared_inputs(inp):
    f = lambda a: np.ascontiguousarray(np.asarray(a, dtype=np.float32))
    w_in = f(inp["w_in"][0])
    perm = np.arange(512).reshape(8, 2, 32)[:, ::-1, :].reshape(512)
    w_in_ext = np.concatenate([w_in, w_in[:, 1536 + perm], w_in[:, 2048 + perm]], axis=1)
    col = lambda g: f(g).reshape(8, 128).T
    gtab = np.concatenate([col(inp["g_mix"][0]), col(inp["g_xattn"][0]), col(inp["g_mem"][0]),
                           col(inp["g_ffn"][0])], axis=1)
    wconvT = f(inp["w_conv"][0]).T.reshape(4, 128, 3).transpose(1, 0, 2).reshape(128, 12)
    d = dict(w_in_ext=f(w_in_ext), w_conv_out=f(inp["w_conv_out"][0]), w_attn_out=f(inp["w_attn_out"][0]),
             w_merge=f(inp["w_merge"][0]), w_xq=f(inp["w_xq"][0]), w_xkv=f(inp["w_xkv"][0]),
             w_xo=f(inp["w_xo"][0]), w_pq=f(inp["w_pq"][0]),
             sub_keys=f(inp["peer_sub_keys"][0]).reshape(2048, 128),
             peer_u=f(inp["peer_u"][0]), peer_v=f(inp["peer_v"][0]),
             gtab=f(gtab), gfin_bc=f(np.broadcast_to(f(inp["g_final"])[None, :], (128, D))),
             wconvT=f(wconvT))
    d.update(_tables())
    return d


def kernel(**inputs):
    n_cores = 8
    x = np.asarray(inputs["x"], dtype=np.float32)
    mem = np.asarray(inputs["mem"], dtype=np.float32)
    B = x.shape[0]
    NB = B // n_cores
    shared = _shared_inputs(inputs)
    nc = build_nc(NB)
    in_maps = []
    for c in range(n_cores):
        m = dict(shared)
        m["x"] = np.ascontiguousarray(x[c * NB:(c + 1) * NB].reshape(NB * SEQ, D))
        m["mem"] = np.ascontiguousarray(mem[c * NB:(c + 1) * NB].reshape(NB * NMEM, D))
        in_maps.append(m)
    res = run_bass_kernel_spmd(nc, in_maps, core_ids=list(range(n_cores)))
    out = np.concatenate([np.asarray(r["out"]).reshape(NB, SEQ, D) for r in res.results], axis=0)
    return out.astype(np.float32)
```

```python
import contextlib
import numpy as np
import ml_dtypes
import concourse.bass as bass
import concourse.mybir as mybir
from concourse.bass_utils import run_bass_kernel_spmd

F32 = mybir.dt.float32
BF16 = mybir.dt.bfloat16
U32 = mybir.dt.uint32
U8 = mybir.dt.uint8
AF = mybir.ActivationFunctionType
ALU = mybir.AluOpType
AX = mybir.AxisListType

PE, ACT, DVE, POOL, SP = "tensor", "scalar", "vector", "gpsimd", "sync"
ENGS = [PE, ACT, DVE, POOL, SP]
N_DMA_SEMS = 24

D = 1024
SEQ = 2048
NMEM = 256
NEXP = 16384
TILE = 512
EPS = 1e-6
NEGB = 640.0


class Buf:
    __slots__ = ("name", "last_write", "reads", "excl")

    def __init__(self, name="", excl=False):
        self.name = name
        self.last_write = None
        self.reads = []
        self.excl = excl


class Op:
    __slots__ = ("eng", "fn", "deps", "is_dma", "signal", "sig_val", "dma_sem", "dma_val")

    def __init__(self, eng, fn, is_dma):
        self.eng = eng
        self.fn = fn
        self.is_dma = is_dma
        self.deps = []
        self.signal = False
        self.sig_val = None
        self.dma_sem = None
        self.dma_val = None


class Prog:
    def __init__(self, nc):
        self.nc = nc
        self.ops = []
        self.n_dma = {SP: 0, POOL: 0}
        self.last_eng = {}
        self.last_dma = {}
        self.bar_ops = []
        self.bar_pending = set()

    def barrier(self):
        self.bar_ops = list(self.last_eng.values()) + list(self.last_dma.values())
        self.bar_pending = set(ENGS)

    def op(self, eng, fn, reads=(), writes=(), dma=False):
        o = Op(eng, fn, dma)
        deps = []
        ex = [b for b in reads if b.excl]
        if ex:
            reads = [b for b in reads if not b.excl]
            writes = list(writes) + ex
        if eng in self.bar_pending:
            self.bar_pending.discard(eng)
            deps.extend(self.bar_ops)
        for b in reads:
            if b.last_write is not None:
                deps.append(b.last_write)
        for b in writes:
            if b.last_write is not None:
                deps.append(b.last_write)
            deps.extend(b.reads)
        seen = set()
        for d in deps:
            if d is o or id(d) in seen:
                continue
            seen.add(id(d))
            if (not d.is_dma) and (not dma) and d.eng == PE and eng == PE:
                continue
            o.deps.append(d)
            if not d.is_dma:
                d.signal = True
        for b in reads:
            if not dma:
                b.reads = [r for r in b.reads if r.is_dma or r.eng != eng]
            b.reads.append(o)
        for b in writes:
            b.last_write = o
            b.reads = []
        if dma:
            base, cnt = (0, 16) if eng == SP else (16, 8)
            k = self.n_dma[eng]
            o.dma_sem = base + k % cnt
            o.dma_val = 16 * (k // cnt + 1)
            self.n_dma[eng] = k + 1
            self.last_dma[o.dma_sem] = o
        else:
            self.last_eng[eng] = o
        self.ops.append(o)
        return o

    def emit(self):
        nc = self.nc
        cnt = {e: 0 for e in ENGS}
        for o in self.ops:
            if not o.is_dma and o.signal:
                cnt[o.eng] += 1
                o.sig_val = cnt[o.eng]
        with contextlib.ExitStack() as st:
            esem = {e: st.enter_context(nc.semaphore("s_" + e)) for e in ENGS}
            dsem = [st.enter_context(nc.semaphore("d_%d" % i)) for i in range(N_DMA_SEMS)]
            block = st.enter_context(nc.Block())
            per_eng = {e: [o for o in self.ops if o.eng == e] for e in ENGS}
            last_dma = {s: o.dma_val for s, o in self.last_dma.items()}

            def make(e):
                def body(eng):
                    waited = {}

                    def wait(sem, key, val):
                        if waited.get(key, 0) >= val:
                            return
                        waited[key] = val
                        eng.wait_ge(sem, val)

                    for o in per_eng[e]:
                        if o.is_dma and o.dma_val > 16:
                            wait(dsem[o.dma_sem], ("d", o.dma_sem), o.dma_val - 16)
                        for d in o.deps:
                            if d.is_dma:
                                wait(dsem[d.dma_sem], ("d", d.dma_sem), d.dma_val)
                            else:
                                wait(esem[d.eng], ("e", d.eng), d.sig_val)
                        ins = o.fn(eng)
                        if o.is_dma:
                            ins.then_inc(dsem[o.dma_sem], 16)
                        elif o.signal:
                            ins.then_inc(esem[o.eng], 1)
                    if e == SP:
                        for s, v in last_dma.items():
                            wait(dsem[s], ("d", s), v)
                return body

            for e in ENGS:
                getattr(block, e)(make(e))


_DT_SIZE = {F32: 4, BF16: 2, U32: 4, U8: 1}


class Arena:
    def __init__(self, nc, name, nbytes):
        self.t = nc.alloc_sbuf_tensor(name, [128, nbytes], U8)
        self.n = nbytes
        self.off = 0

    def alloc(self, shape, dt):
        sz = _DT_SIZE[dt]
        n = int(np.prod(shape)) * sz
        off = (self.off + 63) // 64 * 64
        assert off + n <= self.n, ("arena overflow", off, n, self.n)
        self.off = off + n
        v = self.t[:, off:off + n].bitcast(dt)
        if len(shape) == 2:
            v = v.rearrange("p (a b) -> p a b", a=shape[0])
        elif len(shape) == 3:
            v = v.rearrange("p (a b c) -> p a b c", a=shape[0], b=shape[1])
        return v


class _Stop(Exception):
    pass


def build_nc(NB, dbg=False, stop=None, skip_c=False):
    nc = bass.Bass("TRN2", target_bir_lowering=False)
    NT = NB * SEQ

    def din(name, shape, dt=F32):
        return nc.dram_tensor(name, shape, dt, kind="ExternalInput").ap()

    x_d = din("x", [NT, D])
    mem_d = din("mem", [NB * NMEM, D])
    win_d = din("w_in_ext", [D, 6144])
    wco_d = din("w_conv_out", [512, D])
    wao_d = din("w_attn_out", [512, D])
    wm_d = din("w_merge", [D, D])
    wxq_d = din("w_xq", [D, D])
    wxkv_d = din("w_xkv", [D, 2048])
    wxo_d = din("w_xo", [D, D])
    wpq_d = din("w_pq", [D, 2048])
    sk_d = din("sub_keys", [2048, 128])
    u_d = din("peer_u", [NEXP, D])
    v_d = din("peer_v", [NEXP, D])
    gtab_d = din("gtab", [128, 32])
    gfin_d = din("gfin_bc", [128, D])
    wconv_d = din("wconvT", [128, 12])
    cos_d = din("cosT", [128, SEQ])
    sin_d = din("sinT", [128, SEQ])
    past_d = din("past64", [SEQ, 64])
    neg_d = din("neg64", [SEQ, 64])
    own_d = din("own64", [SEQ, 64])
    tri_d = din("tri", [128, 128])
    eb_d = din("eb", [8, 1024])
    id_d = din("ident", [128, 128])
    iota_d = din("iota128", [128, 128])
    iota16_d = din("iota16", [128, 16])
    iorep_d = din("iorep", [128, 128 * 16])
    out_d = nc.dram_tensor("out", [NT, D], F32, kind="ExternalOutput").ap()
    dbg_d = {}
    if dbg:
        for nm in ("h1", "h2"):
            dbg_d[nm] = nc.dram_tensor("dbg_" + nm, [NT // TILE, 128, 8 * TILE], F32,
                                       kind="ExternalOutput").ap()
        dbg_d["pe"] = nc.dram_tensor("dbg_pe", [NT, D], F32, kind="ExternalOutput").ap()
        dbg_d["bias"] = nc.dram_tensor("dbg_bias", [NT, 64], BF16, kind="ExternalOutput").ap()
        dbg_d["gate"] = nc.dram_tensor("dbg_gate", [NT, 64], F32, kind="ExternalOutput").ap()
        dbg_d["o"] = nc.dram_tensor("dbg_o", [NT, 512], BF16, kind="ExternalOutput").ap()

    uT_s = nc.dram_tensor("uT_s", [128, 8, NEXP], BF16).ap()
    v_s = nc.dram_tensor("v_s", [NEXP, D], BF16).ap()
    win_s = nc.dram_tensor("win_s", [D, 6144], BF16).ap()
    wco_s = nc.dram_tensor("wco_s", [512, D], BF16).ap()
    wao_s = nc.dram_tensor("wao_s", [512, D], BF16).ap()
    wm_s = nc.dram_tensor("wm_s", [D, D], BF16).ap()
    wxq_s = nc.dram_tensor("wxq_s", [D, D], BF16).ap()
    wxkv_s = nc.dram_tensor("wxkv_s", [D, 2048], BF16).ap()
    wxo_s = nc.dram_tensor("wxo_s", [D, D], BF16).ap()
    wpq_s = nc.dram_tensor("wpq_s", [D, 2048], BF16).ap()
    mkT_s = nc.dram_tensor("mkT_s", [128, 8 * NMEM], BF16).ap()
    mv_s = nc.dram_tensor("mv_s", [128, 2 * D], BF16).ap()

    P = Prog(nc)

    def sbt(name, shape, dt):
        return nc.alloc_sbuf_tensor(name, shape, dt)

    ident32 = sbt("ident32", [128, 128], F32)
    identb = sbt("identb", [128, 128], BF16)
    onesb = sbt("onesb", [128, 128], BF16)
    iota128 = sbt("iota128s", [128, 128], F32)
    iota16 = sbt("iota16s", [128, 16], F32)
    trib = sbt("trib", [128, 128], BF16)
    gtab = sbt("gtabs", [128, 32], F32)
    wconv = sbt("wconvs", [128, 12], F32)
    ebb = sbt("ebb", [8, 8, 128], BF16)
    hT = sbt("hT", [128, 8, TILE], F32)
    KT = sbt("KT", [128, 4, SEQ], BF16)
    V = sbt("V", [128, 16, 8, 65], BF16)
    kbarT = sbt("kbarT", [128, 4, 8], BF16)
    uhalo = sbt("uhalo", [128, 4, 2], F32)
    B_const = Buf("const")
    B_hT = Buf("hT")
    B_KT = Buf("KT")
    B_V = Buf("V")
    B_kbar = Buf("kbar")
    B_uh = Buf("uhalo")
    B_out = Buf("out")
    B_uTs = Buf("uT_s")
    B_vs = Buf("v_s")
    B_mks = Buf("mk_s")
    B_mvs = Buf("mv_s")

    ARENA_BYTES = 141 * 1024
    arena = Arena(nc, "arena", ARENA_BYTES)
    banks = [nc.alloc_psum_tensor("pb%d" % i, [128, 512], F32) for i in range(8)]
    B_bank = [Buf("pb%d" % i, excl=True) for i in range(8)]

    def bank_bf(i):
        return banks[i][:].bitcast(BF16)

    def mm(out, lhsT, rhs, start, stop, r, w, skip=False):
        if skip:
            P.op(PE, lambda e: e.matmul(out, lhsT, rhs, start=start, stop=stop,
                                        skip_group_check=True), reads=r, writes=w)
        else:
            P.op(PE, lambda e: e.matmul(out, lhsT, rhs, start=start, stop=stop), reads=r, writes=w)

    def tr(out, in_, ident, r, w):
        P.op(PE, lambda e: e.transpose(out, in_, ident), reads=r, writes=w)

    def act(out, in_, func, r, w, bias=None, scale=None, accum=None):
        kw = {}
        if bias is not None:
            kw["bias"] = bias
        if scale is not None:
            kw["scale"] = scale
        if accum is not None:
            kw["accum_out"] = accum
        P.op(ACT, lambda e: e.activation(out, in_, func, **kw), reads=r, writes=w)

    def tt(eng, out, in0, in1, op, r, w):
        P.op(eng, lambda e: e.tensor_tensor(out, in0, in1, op), reads=r, writes=w)

    def ts(eng, out, in0, s1, s2, op0, op1, r, w):
        if op1 is None:
            P.op(eng, lambda e: e.tensor_scalar(out, in0, s1, None, op0), reads=r, writes=w)
        else:
            P.op(eng, lambda e: e.tensor_scalar(out, in0, s1, s2, op0, op1), reads=r, writes=w)

    def stt(eng, out, in0, scalar, in1, op0, op1, r, w):
        P.op(eng, lambda e: e.scalar_tensor_tensor(out, in0, scalar, in1, op0, op1), reads=r, writes=w)

    def cp(eng, out, in_, r, w):
        if eng == ACT:
            P.op(ACT, lambda e: e.activation(out, in_, AF.Copy), reads=r, writes=w)
        else:
            P.op(eng, lambda e: e.tensor_copy(out, in_), reads=r, writes=w)

    def dma(eng, out, in_, r, w):
        P.op(eng, lambda e: e.dma_start(out=out, in_=in_), reads=r, writes=w, dma=True)

    def memset(eng, ap, val, w):
        P.op(eng, lambda e: e.memset(ap, val), writes=w)

    dma(SP, ident32[:], id_d, [], [B_const])
    dma(POOL, identb[:], id_d, [], [B_const])
    dma(SP, iota128[:], iota_d, [], [B_const])
    dma(SP, iota16[:], iota16_d, [], [B_const])
    dma(POOL, trib[:], tri_d, [], [B_const])
    dma(SP, gtab[:], gtab_d, [], [B_const])
    dma(SP, wconv[:], wconv_d, [], [B_const])
    dma(POOL, ebb[:], eb_d.rearrange("n (b k) -> n b k", b=8), [], [B_const])
    memset(DVE, onesb[:], 1.0, [B_const])
    memset(DVE, V[:], 1.0, [B_V])

    for (dst, src, rows) in ((wxkv_s, wxkv_d, D), (win_s, win_d, D), (wco_s, wco_d, 512), (wao_s, wao_d, 512),
                             (wm_s, wm_d, D), (wxq_s, wxq_d, D), (wxo_s, wxo_d, D), (wpq_s, wpq_d, D)):
        for r0 in range(0, rows, 256):
            dma(POOL, dst[r0:r0 + 256, :], src[r0:r0 + 256, :], [], [])
    ub = [sbt("ub%d" % i, [128, D], BF16) for i in range(2)]
    B_ub = [Buf("ub0"), Buf("ub1")]
    uts = [sbt("uts%d" % i, [128, 8, 128], BF16) for i in range(2)]
    B_uts = [Buf("uts0"), Buf("uts1")]
    prep_state = [0]

    def prep_slice(n):
        while n > 0 and prep_state[0] < NEXP // 128:
            i = prep_state[0]
            prep_state[0] += 1
            n -= 1
            s = i % 2
            if i % 16 == 0:
                r0 = (i // 16) * 2048
                dma(POOL, v_s[r0:r0 + 2048, :], v_d[r0:r0 + 2048, :], [], [])
            dma(POOL, ub[s][:], u_d[i * 128:(i + 1) * 128, :], [], [B_ub[s]])
            bk = 2 + i % 2
            pv = bank_bf(bk)
            for kc in range(8):
                tr(pv[:, kc * 128:(kc + 1) * 128], ub[s][:, kc * 128:(kc + 1) * 128], identb[:],
                   [B_ub[s], B_const], [B_bank[bk]])
            cp(ACT if i % 2 == 0 else DVE, uts[s][:],
               pv.rearrange("p (a b) -> p a b", a=8), [B_bank[bk]], [B_uts[s]])
            dma(SP, uT_s[:, :, i * 128:(i + 1) * 128], uts[s][:], [B_uts[s]], [])

    arena.off = 0
    prep_slice(4)

    skT = sbt("skT", [128, 16, 128], BF16)
    B_skT = Buf("skT")
    skl = arena.alloc([16, 128], BF16)
    B_skl = Buf("skl")
    dma(POOL, skl, sk_d.rearrange("(c n) d -> n c d", n=128), [], [B_skl])
    for c in range(16):
        bk = 2 + c // 8
        pv = bank_bf(bk)
        tr(pv[:, (c % 8) * 128:(c % 8 + 1) * 128], skl[:, c, :], identb[:], [B_skl, B_const], [B_bank[bk]])
        if c % 8 == 7:
            cp(DVE, skT[:, c - 7:c + 1, :], pv.rearrange("p (a b) -> p a b", a=8), [B_bank[bk]], [B_skT])

    def norm_fm(src, B_src, N, gcol0, aT, B_aT, sq, B_sq, rs1, rs2, B_rs, bk):
        for kc in range(8):
            s = kc % 2
            act(sq[s][:, :N], src[:, kc, :N], AF.Square, [B_src], [B_sq[s]])
            mm(banks[bk][:, :N], onesb[:], sq[s][:, :N], kc == 0, kc == 7, [B_sq[s], B_const], [B_bank[bk]])
        act(rs1[:, :N], banks[bk][:, :N], AF.Sqrt, [B_bank[bk]], [B_rs[0]], bias=EPS, scale=1.0 / D)
        P.op(DVE, lambda e: e.reciprocal(rs2[:, :N], rs1[:, :N]), reads=[B_rs[0]], writes=[B_rs[1]])
        for kc in range(8):
            stt(DVE, aT[:, kc, :N], src[:, kc, :N],
                gtab[:, gcol0 + kc:gcol0 + kc + 1], rs2[:, :N], ALU.mult, ALU.mult,
                [B_src, B_rs[1], B_const], [B_aT])

    class WStream:
        def __init__(self, nbuf, kc, ncols):
            self.bufs = [arena.alloc([kc, ncols], BF16) for _ in range(nbuf)]
            self.B = [Buf("w%d" % i) for i in range(nbuf)]
            self.i = 0

        def load(self, src, kc):
            s = self.i % len(self.bufs)
            self.i += 1
            dma(POOL, self.bufs[s][:, :kc, :], src.rearrange("(kc p) n -> p kc n", p=128), [], [self.B[s]])
            return self.bufs[s], self.B[s]

    def proj_fm(w, B_w, col0, aT, B_aT, N, bk, kcs=8):
        for kc in range(kcs):
            mm(banks[bk][:, :N], w[:, kc, col0:col0 + 128], aT[:, kc, :N], kc == 0, kc == kcs - 1,
               [B_w, B_aT], [B_bank[bk]])

    def chk(k):
        if stop == k:
            raise _Stop()

    try:
     chk(0)
     for bi in range(NB):
         P.barrier()
         arena.off = 0
         ws = WStream(3, 8, 512)
         memT = arena.alloc([8, NMEM], F32)
         B_memT = Buf("memT")
         mtm = [arena.alloc([D], F32) for _ in range(2)]
         B_mtm = [Buf("mtm0"), Buf("mtm1")]
         mnT = arena.alloc([8, NMEM], BF16)
         B_mnT = Buf("mnT")
         sq = [arena.alloc([TILE], BF16) for _ in range(2)]
         B_sq = [Buf("sq0"), Buf("sq1")]
         rs1 = arena.alloc([TILE], F32)
         rs2 = arena.alloc([TILE], F32)
         B_rs = [Buf("rs1"), Buf("rs2")]
         mk_sb = arena.alloc([8, NMEM], BF16)
         B_mk = Buf("mk_sb")
         mv_sb = arena.alloc([2, D], BF16)
         B_mv = Buf("mv_sb")
         for sub in range(2):
             r0 = bi * NMEM + sub * 128
             dma(SP, mtm[sub], mem_d[r0:r0 + 128, :], [], [B_mtm[sub]])
             for half in range(2):
                 bk = half
                 for j in range(4):
                     kc = half * 4 + j
                     tr(banks[bk][:, j * 128:(j + 1) * 128], mtm[sub][:, kc * 128:(kc + 1) * 128], ident32[:],
                        [B_mtm[sub], B_const], [B_bank[bk]])
                 cp(ACT if half == 0 else DVE, memT[:, half * 4:half * 4 + 4, sub * 128:(sub + 1) * 128],
                    banks[bk][:].rearrange("p (a b) -> p a b", a=4), [B_bank[bk]], [B_memT])
         norm_fm(memT, B_memT, NMEM, 16, mnT, B_mnT, sq, B_sq, rs1, rs2, B_rs, 2)
         for g in range(4):
             w, B_w = ws.load(wxkv_s[:, g * 512:(g + 1) * 512], 8)
             if g < 2:
                 for cc in range(4):
                     c = g * 4 + cc
                     bk = c % 2
                     proj_fm(w, B_w, cc * 128, mnT, B_mnT, NMEM, bk)
                     cp(ACT if c % 2 == 0 else DVE, mk_sb[:, c, :], banks[bk][:, :NMEM], [B_bank[bk]], [B_mk])
             else:
                 for mc in range(2):
                     bk = 2 + mc
                     for kc in range(8):
                         mm(banks[bk][:, :], mnT[:, kc, mc * 128:(mc + 1) * 128], w[:, kc, :], kc == 0, kc == 7,
                            [B_w, B_mnT], [B_bank[bk]])
                     cp(ACT if mc == 0 else DVE, mv_sb[:, mc, (g - 2) * 512:(g - 1) * 512], banks[bk][:, :],
                        [B_bank[bk]], [B_mv])
         dma(SP, mkT_s.rearrange("p (c m) -> p c m", c=8), mk_sb, [B_mk], [B_mks])
         dma(SP, mv_s.rearrange("p (c m) -> p c m", c=2), mv_sb, [B_mv], [B_mvs])
         memset(DVE, kbarT[:], 0.0, [B_kbar])
         memset(DVE, uhalo[:], 0.0, [B_uh])
         chk(1)
         prep_slice(8)

         for qi in range(SEQ // TILE):
             tg = bi * (SEQ // TILE) + qi
             t0 = bi * SEQ + qi * TILE
             p0 = qi * TILE

             P.barrier()
             arena.off = 0
             ws = WStream(3, 8, 512)
             wco = arena.alloc([4, D], BF16)
             wao = arena.alloc([4, D], BF16)
             wmr = arena.alloc([8, D], BF16)
             B_wco, B_wao, B_wmr = Buf("wco"), Buf("wao"), Buf("wmr")
             aT = arena.alloc([8, TILE], BF16)
             B_aT = Buf("aT")
             sq = [arena.alloc([TILE], BF16) for _ in range(2)]
             B_sq = [Buf("sq0"), Buf("sq1")]
             rs1 = arena.alloc([TILE], F32)
             rs2 = arena.alloc([TILE], F32)
             B_rs = [Buf("rs1"), Buf("rs2")]
             cosb = arena.alloc([TILE], F32)
             sinb = arena.alloc([TILE], F32)
             B_cs = Buf("cossin")
             pastb = arena.alloc([4, 64], F32)
             negb = arena.alloc([4, 64], F32)
             ownb = arena.alloc([4, 64], F32)
             B_msk = Buf("masks")
             QT = arena.alloc([4, TILE], BF16)
             B_QT = Buf("QT")
             t1 = [arena.alloc([TILE], F32) for _ in range(2)]
             t2 = [arena.alloc([TILE], F32) for _ in range(2)]
             B_t1 = [Buf("t1a"), Buf("t1b")]
             B_t2 = [Buf("t2a"), Buf("t2b")]
             kb32 = arena.alloc([4, 2], F32)
             B_kb32 = Buf("kb32")
             gate_sb = arena.alloc([64], F32)
             gm = arena.alloc([64], F32)
             thr8 = arena.alloc([8, 8], F32)
             sel = arena.alloc([64], F32)
             biasb = arena.alloc([4, 64], BF16)
             B_gate, B_gm, B_thr, B_sel, B_biasb = Buf("gate"), Buf("gm"), Buf("thr"), Buf("sel"), Buf("biasb")
             biasT = [arena.alloc([TILE], BF16) for _ in range(2)]
             B_biasT = [Buf("biasT0"), Buf("biasT1")]
             PT = [arena.alloc([TILE], BF16) for _ in range(4)]
             B_PT = [Buf("PT%d" % i) for i in range(4)]
             rinv = [arena.alloc([4], F32) for _ in range(2)]
             B_rinv = [Buf("rinv0"), Buf("rinv1")]
             o_tm = arena.alloc([4, 512], BF16)
             B_otm = Buf("o_tm")
             oT = arena.alloc([4, TILE], BF16)
             B_oT = Buf("oT")
             xs = [arena.alloc([TILE], F32) for _ in range(2)]
             B_xs = [Buf("xs0"), Buf("xs1")]
             cv = [arena.alloc([TILE], F32) for _ in range(2)]
             B_cv = [Buf("cv0"), Buf("cv1")]
             yc = arena.alloc([4, TILE], BF16)
             B_yc = Buf("yc")
             merged = arena.alloc([8, TILE], BF16)
             B_mrg = Buf("merged")
             sgblk = arena.alloc([4, TILE], F32)
             sg = [sgblk[:, i, :] for i in range(4)]
             B_sg = [Buf("sg%d" % i) for i in range(4)]
             uconv = arena.alloc([4, TILE + 2], F32)
             B_u = Buf("uconv")
             cp(POOL, uconv[:, :, 0:2], uhalo[:], [B_uh], [B_u])
             xtm = [sgblk[:, 2 * i:2 * i + 2, :].rearrange("p a b -> p (a b)") for i in range(2)]
             B_xtm = [Buf("xtm0"), Buf("xtm1")]

             dma(SP, cosb, cos_d[:, p0:p0 + TILE], [], [B_cs])
             dma(SP, sinb, sin_d[:, p0:p0 + TILE], [], [B_cs])
             dma(SP, pastb, past_d[p0:p0 + TILE, :].rearrange("(s p) n -> p s n", p=128), [], [B_msk])
             dma(SP, negb, neg_d[p0:p0 + TILE, :].rearrange("(s p) n -> p s n", p=128), [], [B_msk])
             dma(SP, ownb, own_d[p0:p0 + TILE, :].rearrange("(s p) n -> p s n", p=128), [], [B_msk])
             pre_k = [ws.load(win_s[:, 2048:2560], 8), ws.load(win_s[:, 5632:6144], 8)]
             for sub in range(4):
                 s = sub % 2
                 dma(SP, xtm[s], x_d[t0 + sub * 128:t0 + (sub + 1) * 128, :], [], [B_xtm[s]])
                 for half in range(2):
                     bk = (sub * 2 + half) % 4
                     for j in range(4):
                         kc = half * 4 + j
                         tr(banks[bk][:, j * 128:(j + 1) * 128], xtm[s][:, kc * 128:(kc + 1) * 128], ident32[:],
                            [B_xtm[s], B_const], [B_bank[bk]])
                     cp(ACT if half == 0 else DVE, hT[:, half * 4:half * 4 + 4, sub * 128:(sub + 1) * 128],
                        banks[bk][:].rearrange("p (a b) -> p a b", a=4), [B_bank[bk]], [B_hT])


             norm_fm(hT, B_hT, TILE, 0, aT, B_aT, sq, B_sq, rs1, rs2, B_rs, 0)

             chk(2.5)
             prep_slice(6)
             def rope_pair(col_a, col_b, dst, B_dst, dst_off, pre=None):
                 if pre is not None:
                     (wa, B_wa), (wb, B_wb) = pre
                 else:
                     wa, B_wa = ws.load(win_s[:, col_a:col_a + 512], 8)
                     wb, B_wb = ws.load(win_s[:, col_b:col_b + 512], 8)
                 for c in range(4):
                     s = c % 2
                     proj_fm(wa, B_wa, c * 128, aT, B_aT, TILE, 0 + s)
                     proj_fm(wb, B_wb, c * 128, aT, B_aT, TILE, 2 + s)
                     tt(DVE, t1[s], banks[0 + s][:, :], cosb, ALU.mult, [B_bank[0 + s], B_cs], [B_t1[s]])
                     tt(DVE, t2[s], banks[2 + s][:, :], sinb, ALU.mult, [B_bank[2 + s], B_cs], [B_t2[s]])
                     tt(DVE, dst[:, c, dst_off:dst_off + TILE], t1[s], t2[s], ALU.add,
                        [B_t1[s], B_t2[s]], [B_dst])

             rope_pair(2048, 5632, KT, B_KT, p0, pre=pre_k)
             prep_slice(6)
             wv, B_wv = ws.load(win_s[:, 2560:3072], 8)
             for sub in range(4):
                 bk = sub % 2
                 for kc in range(8):
                     mm(banks[bk][:, :], aT[:, kc, sub * 128:(sub + 1) * 128], wv[:, kc, :], kc == 0, kc == 7,
                        [B_wv, B_aT], [B_bank[bk]])
                 cp(ACT if sub % 2 == 0 else DVE, V[:, qi * 4 + sub, :, 0:64],
                    banks[bk][:].rearrange("p (h d) -> p h d", h=8), [B_bank[bk]], [B_V])
             rope_pair(1536, 5120, QT, B_QT, 0)
             prep_slice(6)
             dma(POOL, wco, wco_s.rearrange("(kc p) n -> p kc n", p=128), [], [B_wco])
             dma(POOL, wao, wao_s.rearrange("(kc p) n -> p kc n", p=128), [], [B_wao])
             dma(POOL, wmr, wm_s.rearrange("(kc p) n -> p kc n", p=128), [], [B_wmr])

             chk(3)
             for c in range(4):
                 P.op(DVE, (lambda o_, i_: lambda e: e.tensor_reduce(o_, i_, AX.X, ALU.add))(
                     kb32[:, c, :], KT[:, c, p0:p0 + TILE].rearrange("p (b k) -> p b k", b=2)),
                     reads=[B_KT], writes=[B_kb32])
             ts(DVE, kbarT[:, :, 2 * qi:2 * qi + 2], kb32, 1.0 / 256.0, None, ALU.mult, None, [B_kb32], [B_kbar])

             for sub in (range(4) if qi >= 2 else ()):
                 for h in range(8):
                     pb = (h % 2) * 64
                     mm(banks[4][:, h * 8:(h + 1) * 8], QT[pb:pb + 64, h // 2, sub * 128:(sub + 1) * 128],
                        kbarT[pb:pb + 64, h // 2, :], True, True, [B_QT, B_kbar], [B_bank[4]])
                 cp(ACT, gate_sb, banks[4][:, 0:64], [B_bank[4]], [B_gate])
                 tt(DVE, gm, gate_sb, pastb[:, sub, :], ALU.mult, [B_gate, B_msk], [B_gm])
                 tt(DVE, gm, gm, negb[:, sub, :], ALU.add, [B_gm, B_msk], [B_gm])
                 for h in range(8):
                     P.op(DVE, (lambda h: lambda e: e.max(out=thr8[:, h, :], in_=gm[:, h * 8:(h + 1) * 8]))(h),
                          reads=[B_gm], writes=[B_thr])
                 for h in range(8):
                     ts(DVE, sel[:, h * 8:(h + 1) * 8], gm[:, h * 8:(h + 1) * 8], thr8[:, h, 2:3], None,
                        ALU.is_ge, None, [B_gm, B_thr], [B_sel])
                 tt(DVE, sel, sel, pastb[:, sub, :], ALU.mult, [B_sel, B_msk], [B_sel])
                 tt(DVE, sel, sel, ownb[:, sub, :], ALU.add, [B_sel, B_msk], [B_sel])
                 ts(DVE, biasb[:, sub, :], sel, -1.0, NEGB, ALU.add, ALU.mult, [B_sel], [B_biasb])
                 if dbg:
                     dma(SP, dbg_d["bias"][t0 + sub * 128:t0 + (sub + 1) * 128, :], biasb[:, sub, :], [B_biasb], [])
                     dma(SP, dbg_d["gate"][t0 + sub * 128:t0 + (sub + 1) * 128, :], gm, [B_gm], [])

             chk(3.3)
             prep_slice(4)
             nkt = 4 * (qi + 1)
             def attend(h):
                 pb = (h % 2) * 64
                 hs = h % 2
                 pvb = bank_bf(4 + hs)
                 if qi >= 2:
                     for sub in range(4):
                         tr(pvb[0:8, sub * 128:(sub + 1) * 128], biasb[:, sub, h * 8:(h + 1) * 8], identb[:],
                            [B_biasb, B_const], [B_bank[4 + hs]])
                     cp(DVE, biasT[hs][0:8, :], pvb[0:8, 0:TILE], [B_bank[4 + hs]], [B_biasT[hs]])
                 ob = 6 + hs
                 O3 = banks[ob][:, 0:260].rearrange("p (j d) -> p j d", j=4)

                 def s_stage(kt):
                     sb_ = 2 * hs + kt % 2
                     c0 = max(0, kt - 4 * qi) * 128
                     need_bias = qi >= 2 and kt < 4 * qi + 2
                     mm(banks[sb_][:, c0:TILE], KT[pb:pb + 64, h // 2, kt * 128:(kt + 1) * 128],
                        QT[pb:pb + 64, h // 2, c0:TILE], True, not need_bias, [B_KT, B_QT], [B_bank[sb_]])
                     if need_bias:
                         mm(banks[sb_][:, c0:TILE], ebb[0:8, kt // 2, :], biasT[hs][0:8, c0:TILE], False, True,
                            [B_const, B_biasT[hs]], [B_bank[sb_]])
                     pt = 2 * hs + kt % 2
                     act(PT[pt][:, c0:TILE], banks[sb_][:, c0:TILE], AF.Exp, [B_bank[sb_]], [B_PT[pt]], scale=0.125)
                     if kt >= 4 * qi:
                         tt(DVE, PT[pt][:, c0:c0 + 128], PT[pt][:, c0:c0 + 128], trib[:], ALU.mult,
                            [B_PT[pt], B_const], [B_PT[pt]])

                 def pv_stage(kt):
                     pt = 2 * hs + kt % 2
                     j0 = max(0, kt - 4 * qi)
                     for j in range(j0, 4):
                         first = (kt == 0 and j == j0)
                         last = (kt == nkt - 1 and j == 3)
                         mm(O3[:, j, :], PT[pt][:, j * 128:(j + 1) * 128], V[:, kt, h, :], first, last,
                            [B_PT[pt], B_V], [B_bank[ob]], skip=True)

                 s_stage(0)
                 yield
                 for kt in range(nkt):
                     if kt + 1 < nkt:
                         s_stage(kt + 1)
                         yield
                     pv_stage(kt)
                     yield
                 P.op(DVE, (lambda O3=O3, hs=hs: lambda e: e.reciprocal(rinv[hs], O3[:, :, 64]))(),
                      reads=[B_bank[ob]], writes=[B_rinv[hs]])
                 for j in range(4):
                     if j % 2 == 0:
                         ts(DVE, o_tm[:, j, h * 64:(h + 1) * 64], O3[:, j, 0:64], rinv[hs][:, j:j + 1], None,
                            ALU.mult, None, [B_bank[ob], B_rinv[hs]], [B_otm])
                     else:
                         act(o_tm[:, j, h * 64:(h + 1) * 64], O3[:, j, 0:64], AF.Copy,
                             [B_bank[ob], B_rinv[hs]], [B_otm], scale=rinv[hs][:, j:j + 1])

             for hp in range(4):
                 prep_slice(8)
                 gens = [attend(2 * hp), attend(2 * hp + 1)]
                 while gens:
                     for g_ in list(gens):
                         try:
                             next(g_)
                         except StopIteration:
                             gens.remove(g_)
             if dbg:
                 dma(SP, dbg_d["o"][t0:t0 + TILE, :].rearrange("(j p) n -> p j n", p=128), o_tm, [B_otm], [])
             for j in range(4):
                 bk = 4 + j % 2
                 pvb = bank_bf(bk)
                 for c in range(4):
                     tr(pvb[:, c * 128:(c + 1) * 128], o_tm[:, j, c * 128:(c + 1) * 128], identb[:],
                        [B_otm, B_const], [B_bank[bk]])
                 cp(ACT if j % 2 == 0 else DVE, oT[:, :, j * 128:(j + 1) * 128],
                    pvb[:, 0:512].rearrange("p (a b) -> p a b", a=4), [B_bank[bk]], [B_oT])

             chk(3.6)
             prep_slice(6)
             wx, B_wx = ws.load(win_s[:, 0:512], 8)
             wc, B_wc = ws.load(win_s[:, 1024:1536], 8)
             wbg, B_wbg = ws.load(win_s[:, 512:1024], 8)
             for c in range(4):
                 s = c % 2
                 proj_fm(wx, B_wx, c * 128, aT, B_aT, TILE, 0 + s)
                 proj_fm(wc, B_wc, c * 128, aT, B_aT, TILE, 2 + s)
                 proj_fm(wbg, B_wbg, c * 128, aT, B_aT, TILE, 4 + s)
                 cp(ACT, xs[s], banks[0 + s][:, :], [B_bank[0 + s]], [B_xs[s]])
                 tt(DVE, uconv[:, c, 2:TILE + 2], banks[2 + s][:, :], xs[s], ALU.mult, [B_bank[2 + s], B_xs[s]], [B_u])
                 ts(DVE, cv[s], uconv[:, c, 2:TILE + 2], wconv[:, c * 3 + 2:c * 3 + 3], None, ALU.mult, None,
                    [B_u, B_const], [B_cv[s]])
                 stt(DVE, cv[s], uconv[:, c, 1:TILE + 1], wconv[:, c * 3 + 1:c * 3 + 2], cv[s], ALU.mult, ALU.add,
                     [B_u, B_cv[s], B_const], [B_cv[s]])
                 stt(DVE, cv[s], uconv[:, c, 0:TILE], wconv[:, c * 3:c * 3 + 1], cv[s], ALU.mult, ALU.add,
                     [B_u, B_cv[s], B_const], [B_cv[s]])
                 tt(DVE, yc[:, c, :], banks[4 + s][:, :], cv[s], ALU.mult, [B_bank[4 + s], B_cv[s]], [B_yc])
             cp(POOL, uhalo[:], uconv[:, :, TILE:TILE + 2], [B_u], [B_uh])

             wgs = {}
             for oc in range(8):
                 if oc % 4 == 0:
                     gci = 3072 + (oc // 4) * 512
                     gai = 4096 + (oc // 4) * 512
                     wgs["c"] = ws.load(win_s[:, gci:gci + 512], 8)
                     wgs["a"] = ws.load(win_s[:, gai:gai + 512], 8)
                 wgc, B_wgc = wgs["c"]
                 wga, B_wga = wgs["a"]
                 gb = 4 * (oc % 2)
                 proj_fm(wgc, B_wgc, (oc % 4) * 128, aT, B_aT, TILE, gb + 0)
                 proj_fm(wga, B_wga, (oc % 4) * 128, aT, B_aT, TILE, gb + 1)
                 for cc in range(4):
                     mm(banks[gb + 2][:, :], wco[:, cc, oc * 128:(oc + 1) * 128], yc[:, cc, :], cc == 0, cc == 3,
                        [B_wco, B_yc], [B_bank[gb + 2]])
                 for cc in range(4):
                     mm(banks[gb + 3][:, :], wao[:, cc, oc * 128:(oc + 1) * 128], oT[:, cc, :], cc == 0, cc == 3,
                        [B_wao, B_oT], [B_bank[gb + 3]])
                 act(sg[0], banks[gb + 0][:, :], AF.Sigmoid, [B_bank[gb + 0]], [B_sg[0]])
                 act(sg[1], banks[gb + 1][:, :], AF.Sigmoid, [B_bank[gb + 1]], [B_sg[1]])
                 tt(DVE, sg[2], banks[gb + 2][:, :], sg[0], ALU.mult, [B_bank[gb + 2], B_sg[0]], [B_sg[2]])
                 tt(DVE, sg[3], banks[gb + 3][:, :], sg[1], ALU.mult, [B_bank[gb + 3], B_sg[1]], [B_sg[3]])
                 tt(DVE, merged[:, oc, :], sg[2], sg[3], ALU.add, [B_sg[2], B_sg[3]], [B_mrg])
             for oc in range(8):
                 bk = 4 + oc % 2
                 for kc in range(8):
                     mm(banks[bk][:, :], wmr[:, kc, oc * 128:(oc + 1) * 128], merged[:, kc, :], kc == 0, kc == 7,
                        [B_wmr, B_mrg], [B_bank[bk]])
                 tt(DVE, hT[:, oc, :], banks[bk][:, :], hT[:, oc, :], ALU.add, [B_bank[bk], B_hT], [B_hT])
             if dbg:
                 dma(SP, dbg_d["h1"][tg].rearrange("p (a b) -> p a b", a=8), hT[:], [B_hT], [])

             chk(4)
             prep_slice(10)
             P.barrier()
             arena.off = 0
             ws = WStream(3, 8, 512)
             aT = arena.alloc([8, TILE], BF16)
             B_aT = Buf("aT")
             sq = [arena.alloc([TILE], BF16) for _ in range(2)]
             B_sq = [Buf("sq0"), Buf("sq1")]
             rs1 = arena.alloc([TILE], F32)
             rs2 = arena.alloc([TILE], F32)
             B_rs = [Buf("rs1"), Buf("rs2")]
             mk_sb = arena.alloc([8, NMEM], BF16)
             mv_sb = arena.alloc([2, D], BF16)
             B_mk, B_mv = Buf("mk"), Buf("mv")
             qxT = arena.alloc([8, TILE], BF16)
             B_qx = Buf("qxT")
             PX = [arena.alloc([TILE], BF16) for _ in range(4)]
             B_PX = [Buf("PX%d" % i) for i in range(4)]
             rxi = [arena.alloc([TILE], F32) for _ in range(2)]
             B_rxi = [Buf("rxi0"), Buf("rxi1")]
             oxT = arena.alloc([8, TILE], BF16)
             B_ox = Buf("oxT")
             dma(SP, mk_sb, mkT_s.rearrange("p (c m) -> p c m", c=8), [B_mks], [B_mk])
             dma(SP, mv_sb, mv_s.rearrange("p (c m) -> p c m", c=2), [B_mvs], [B_mv])
             norm_fm(hT, B_hT, TILE, 8, aT, B_aT, sq, B_sq, rs1, rs2, B_rs, 0)
             for g in range(2):
                 w, B_w = ws.load(wxq_s[:, g * 512:(g + 1) * 512], 8)
                 for cc in range(4):
                     c = g * 4 + cc
                     bk = c % 2
                     proj_fm(w, B_w, cc * 128, aT, B_aT, TILE, bk)
                     cp(ACT if c % 2 == 0 else DVE, qxT[:, c, :], banks[bk][:, :], [B_bank[bk]], [B_qx])
             for h in range(4):
                 hs = h % 2
                 for mc in range(2):
                     bk = 2 + mc
                     for dc in range(2):
                         mm(banks[bk][:, :], mk_sb[:, h * 2 + dc, mc * 128:(mc + 1) * 128], qxT[:, h * 2 + dc, :],
                            dc == 0, dc == 1, [B_mk, B_qx], [B_bank[bk]])
                     act(PX[hs * 2 + mc], banks[bk][:, :], AF.Exp, [B_bank[bk]], [B_PX[hs * 2 + mc]], scale=1.0 / 16.0)
                 for mc in range(2):
                     mm(banks[4][:, :], onesb[:], PX[hs * 2 + mc], mc == 0, mc == 1,
                        [B_const, B_PX[hs * 2 + mc]], [B_bank[4]])
                 P.op(DVE, (lambda hs=hs: lambda e: e.reciprocal(rxi[hs], banks[4][:, :]))(),
                      reads=[B_bank[4]], writes=[B_rxi[hs]])
                 for dc in range(2):
                     bk = 5 + dc
                     for mc in range(2):
                         c0 = h * 256 + dc * 128
                         mm(banks[bk][:, :], mv_sb[:, mc, c0:c0 + 128], PX[hs * 2 + mc], mc == 0, mc == 1,
                            [B_mv, B_PX[hs * 2 + mc]], [B_bank[bk]])
                     tt(DVE, oxT[:, h * 2 + dc, :], banks[bk][:, :], rxi[hs], ALU.mult,
                        [B_bank[bk], B_rxi[hs]], [B_ox])
             for g in range(2):
                 w, B_w = ws.load(wxo_s[:, g * 512:(g + 1) * 512], 8)
                 for cc in range(4):
                     oc = g * 4 + cc
                     bk = oc % 2
                     proj_fm(w, B_w, cc * 128, oxT, B_ox, TILE, bk)
                     tt(DVE, hT[:, oc, :], banks[bk][:, :], hT[:, oc, :], ALU.add, [B_bank[bk], B_hT], [B_hT])
             if dbg:
                 dma(SP, dbg_d["h2"][tg].rearrange("p (a b) -> p a b", a=8), hT[:], [B_hT], [])

             chk(5)
             prep_slice(10)
             TC = 256
             for cs in range(0 if skip_c else TILE // TC):
                 c_t0 = cs * TC
                 P.barrier()
                 arena.off = 0
                 aT = arena.alloc([8, TC], BF16)
                 B_aT = Buf("aT")
                 IT = arena.alloc([TC], BF16)
                 JT = arena.alloc([TC], BF16)
                 gT = arena.alloc([TC], BF16)
                 B_IT, B_JT, B_gT = Buf("IT"), Buf("JT"), Buf("gT")
                 iorep = arena.alloc([128, 16], BF16)
                 B_iorep = Buf("iorep")
                 dma(POOL, iorep, iorep_d.rearrange("p (i t) -> p i t", t=16), [], [B_iorep])
                 mark = arena.off
                 ws = WStream(3, 8, 512)
                 sq = [arena.alloc([TILE], BF16) for _ in range(2)]
                 B_sq = [Buf("sq0"), Buf("sq1")]
                 rs1 = arena.alloc([TILE], F32)
                 rs2 = arena.alloc([TILE], F32)
                 B_rs = [Buf("rs1"), Buf("rs2")]
                 pqT = arena.alloc([16, TC], BF16)
                 B_pq = Buf("pqT")
                 class _NS:
                     pass

                 subs = []
                 for _si in range(TC // 128):
                     W = _NS()
                     W.sc = arena.alloc([2048], F32)
                     W.B_sc = Buf("sc")
                     W.scrs = [arena.alloc([128], F32) for _ in range(16)]
                     W.B_scrs = [Buf("scr%d" % i) for i in range(16)]
                     W.B_Va = [Buf("Va%d" % i) for i in range(16)]
                     W.B_Vb = [Buf("Vb%d" % i) for i in range(16)]
                     W.B_Ia = [Buf("Ia%d" % i) for i in range(16)]
                     W.B_Ib = [Buf("Ib%d" % i) for i in range(16)]
                     W.V16 = arena.alloc([16, 16], F32)
                     W.I16 = arena.alloc([16, 16], U32)
                     W.I16f = arena.alloc([16, 16], F32)
                     W.B_I16f = Buf("I16f")
                     W.cand = arena.alloc([8, 16, 16], F32)
                     W.B_cand = Buf("cand")
                     W.cscrs = [arena.alloc([256], F32) for _ in range(8)]
                     W.B_cscrs = [Buf("cscr%d" % i) for i in range(8)]
                     W.B_Ta = [Buf("Ta%d" % i) for i in range(8)]
                     W.B_Tb = [Buf("Tb%d" % i) for i in range(8)]
                     W.B_Pa = [Buf("Pa%d" % i) for i in range(8)]
                     W.B_Pb = [Buf("Pb%d" % i) for i in range(8)]
                     W.T16 = arena.alloc([8, 16], F32)
                     W.P16 = arena.alloc([8, 16], U32)
                     W.K1 = arena.alloc([8, 16], U32)
                     W.K2 = arena.alloc([8, 16], U32)
                     W.K1f = arena.alloc([8, 16], F32)
                     W.K2f = arena.alloc([8, 16], F32)
                     W.B_K = Buf("K12")
                     W.eq = W.cand
                     W.B_eq = Buf("eq")
                     W.nmx = arena.alloc([8], F32)
                     W.zs = arena.alloc([8], F32)
                     W.zr = arena.alloc([8], F32)
                     W.B_nmx, W.B_zs = Buf("nmx"), Buf("zs")
                     W.Itm = arena.alloc([128], F32)
                     W.Jtm = arena.alloc([128], F32)
                     W.gtm = arena.alloc([128], F32)
                     W.B_Itm, W.B_Jtm, W.B_gtm = Buf("Itm"), Buf("Jtm"), Buf("gtm")
                     subs.append(W)

                 norm_fm(hT[:, :, c_t0:c_t0 + TC], B_hT, TC, 24, aT, B_aT, sq, B_sq, rs1, rs2, B_rs, 0)
                 for g in range(4):
                     w, B_w = ws.load(wpq_s[:, g * 512:(g + 1) * 512], 8)
                     for cc in range(4):
                         c = g * 4 + cc
                         bk = 4 + c % 2
                         proj_fm(w, B_w, cc * 128, aT, B_aT, TC, bk)
                         cp(ACT, pqT[:, c, :], banks[bk][:, :TC], [B_bank[bk]], [B_pq])
                 chk(5.1)
                 prep_slice(8)
                 def mk_max(o_, i_):
                     return lambda e: e.max(out=o_, in_=i_)

                 def mk_idx(o_, m_, v_):
                     return lambda e: e.max_index(out=o_, in_max=m_, in_values=v_)

                 def mk_rep(o_, m_, v_):
                     return lambda e: e.match_replace(out=o_, in_to_replace=m_, in_values=v_, imm_value=-1e30)

                 def mk_ss(o_, i_, v_, op_):
                     return lambda e: e.tensor_single_scalar(o_, i_, v_, op_)

                 def mk_rcp(o_, i_):
                     return lambda e: e.reciprocal(o_, i_)

                 def mk_red(o_, i_):
                     return lambda e: e.tensor_reduce(o_, i_, AX.X, ALU.add)

                 def topk_sub(tsub, W):
                     b0 = 4 * tsub
                     sc, V16, I16, I16f, cand, T16, P16 = W.sc, W.V16, W.I16, W.I16f, W.cand, W.T16, W.P16
                     for c in range(16):
                         bk = b0 + c // 4
                         mm(banks[bk][:, (c % 4) * 128:(c % 4 + 1) * 128], pqT[:, c, tsub * 128:(tsub + 1) * 128],
                            skT[:, c, :], True, True, [B_pq, B_skT], [B_bank[bk]])
                     for q4 in range(4):
                         cp(ACT, sc[:, q4 * 512:(q4 + 1) * 512], banks[b0 + q4][:, :],
                            [B_bank[b0 + q4]], [W.B_sc])
                     yield
                     for c in range(16):
                         P.op(DVE, mk_max(V16[:, c, 0:8], sc[:, c * 128:(c + 1) * 128]), reads=[W.B_sc],
                              writes=[W.B_Va[c]])
                     yield
                     for c in range(16):
                         scc = sc[:, c * 128:(c + 1) * 128]
                         P.op(DVE, mk_idx(I16[:, c, 0:8], V16[:, c, 0:8], scc), reads=[W.B_sc, W.B_Va[c]],
                              writes=[W.B_Ia[c]])
                         P.op(DVE, mk_rep(W.scrs[c], V16[:, c, 0:8], scc), reads=[W.B_sc, W.B_Va[c]],
                              writes=[W.B_scrs[c]])
                     yield
                     for c in range(16):
                         P.op(DVE, mk_max(V16[:, c, 8:16], W.scrs[c]), reads=[W.B_scrs[c]], writes=[W.B_Vb[c]])
                     yield
                     for c in range(16):
                         P.op(DVE, mk_idx(I16[:, c, 8:16], V16[:, c, 8:16], W.scrs[c]),
                              reads=[W.B_scrs[c], W.B_Vb[c]], writes=[W.B_Ib[c]])
                     B_V16 = W.B_Va + W.B_Vb
                     B_I16 = W.B_Ia + W.B_Ib
                     cp(DVE, I16f, I16, B_I16, [W.B_I16f])
                     V4 = V16.rearrange("p (h two) k -> p h two k", two=2)
                     I4 = I16f.rearrange("p (h two) k -> p h two k", two=2)
                     tt(DVE, cand, V4[:, :, 0, :].unsqueeze(3).to_broadcast([128, 8, 16, 16]),
                        V4[:, :, 1, :].unsqueeze(2).to_broadcast([128, 8, 16, 16]), ALU.add, B_V16, [W.B_cand])
                     yield
                     chs = [cand[:, h, :, :].rearrange("p a b -> p (a b)") for h in range(8)]
                     for h in range(8):
                         P.op(DVE, mk_max(T16[:, h, 0:8], chs[h]), reads=[W.B_cand], writes=[W.B_Ta[h]])
                     yield
                     for h in range(8):
                         P.op(DVE, mk_idx(P16[:, h, 0:8], T16[:, h, 0:8], chs[h]), reads=[W.B_cand, W.B_Ta[h]],
                              writes=[W.B_Pa[h]])
                         P.op(DVE, mk_rep(W.cscrs[h], T16[:, h, 0:8], chs[h]), reads=[W.B_cand, W.B_Ta[h]],
                              writes=[W.B_cscrs[h]])
                     yield
                     for h in range(8):
                         P.op(DVE, mk_max(T16[:, h, 8:16], W.cscrs[h]), reads=[W.B_cscrs[h]], writes=[W.B_Tb[h]])
                     yield
                     for h in range(8):
                         P.op(DVE, mk_idx(P16[:, h, 8:16], T16[:, h, 8:16], W.cscrs[h]),
                              reads=[W.B_cscrs[h], W.B_Tb[h]], writes=[W.B_Pb[h]])
                     B_T16l = W.B_Ta + W.B_Tb
                     B_P16l = W.B_Pa + W.B_Pb
                     ts(DVE, W.nmx, T16[:, :, 0], -1.0, None, ALU.mult, None, B_T16l, [W.B_nmx])
                     yield
                     for h in range(8):
                         act(W.gtm[:, h * 16:(h + 1) * 16], T16[:, h, :], AF.Exp, B_T16l + [W.B_nmx],
                             [W.B_gtm, W.B_zs], bias=W.nmx[:, h:h + 1], scale=1.0, accum=W.zs[:, h:h + 1])
                     P.op(DVE, mk_ss(W.K1, P16, 4, ALU.logical_shift_right), reads=B_P16l, writes=[W.B_K])
                     P.op(DVE, mk_ss(W.K2, P16, 15, ALU.bitwise_and), reads=B_P16l, writes=[W.B_K])
                     cp(DVE, W.K1f, W.K1, [W.B_K], [W.B_K])
                     cp(DVE, W.K2f, W.K2, [W.B_K], [W.B_K])
                     yield
                     P.op(DVE, mk_rcp(W.zr, W.zs), reads=[W.B_zs], writes=[W.B_nmx])
                     g3v = W.gtm.rearrange("p (h k) -> p h k", h=8)
                     tt(DVE, g3v, g3v, W.zr.unsqueeze(2).to_broadcast([128, 8, 16]), ALU.mult,
                        [W.B_gtm, W.B_nmx], [W.B_gtm])
                     yield
                     io4 = iota16[:].unsqueeze(1).unsqueeze(1).to_broadcast([128, 8, 16, 16])
                     for (Kf, half, dst, B_dst) in ((W.K1f, 0, W.Itm, W.B_Itm), (W.K2f, 1, W.Jtm, W.B_Jtm)):
                         tt(DVE, W.eq, Kf.unsqueeze(3).to_broadcast([128, 8, 16, 16]), io4, ALU.is_equal,
                            [W.B_K, B_const, W.B_cand], [W.B_eq])
                         yield
                         tt(DVE, W.eq, W.eq, I4[:, :, half, :].unsqueeze(2).to_broadcast([128, 8, 16, 16]),
                            ALU.mult, [W.B_eq, W.B_I16f], [W.B_eq])
                         yield
                         P.op(DVE, mk_red(dst.rearrange("p (h k) -> p h k", h=8), W.eq), reads=[W.B_eq],
                              writes=[B_dst])
                         yield
                     tb = b0
                     for (src, B_src, col) in ((W.Itm, W.B_Itm, 0), (W.Jtm, W.B_Jtm, 128), (W.gtm, W.B_gtm, 256)):
                         tr(banks[tb][:, col:col + 128], src, ident32[:], [B_src, B_const], [B_bank[tb]])
                     cp(ACT, IT[:, tsub * 128:(tsub + 1) * 128], banks[tb][:, 0:128], [B_bank[tb]], [B_IT])
                     cp(ACT, JT[:, tsub * 128:(tsub + 1) * 128], banks[tb][:, 128:256], [B_bank[tb]], [B_JT])
                     cp(ACT, gT[:, tsub * 128:(tsub + 1) * 128], banks[tb][:, 256:384], [B_bank[tb]], [B_gT])

                 prep_slice(16)
                 gens = [topk_sub(i, subs[i]) for i in range(TC // 128)]
                 while gens:
                     for g_ in list(gens):
                         try:
                             next(g_)
                         except StopIteration:
                             gens.remove(g_)

                 chk(6)
                 prep_slice(128)
                 P.barrier()
                 arena.off = mark
                 G = arena.alloc([128, TC], BF16)
                 B_G = Buf("G")
                 NI = 4
                 NBUF = 2
                 ubuf = [arena.alloc([8, NI * 128], BF16) for _ in range(NBUF)]
                 vbuf = [arena.alloc([NI, D], BF16) for _ in range(NBUF)]
                 B_ubuf = [Buf("ub%d" % i) for i in range(NBUF)]
                 B_vbuf = [Buf("vb%d" % i) for i in range(NBUF)]
                 actb = [arena.alloc([TC], BF16) for _ in range(4)]
                 gab = [arena.alloc([TC], BF16) for _ in range(4)]
                 B_actb = [Buf("act%d" % i) for i in range(4)]
                 B_gab = [Buf("ga%d" % i) for i in range(4)]
                 TB = 16
                 oi_off = arena.off
                 OI = [arena.alloc([128, TB], BF16) for _ in range(2)]
                 OJ = [arena.alloc([128, TB], BF16) for _ in range(2)]
                 B_OI = [Buf("OI%d" % i) for i in range(2)]
                 B_OJ = [Buf("OJ%d" % i) for i in range(2)]
                 for b in range(TC // TB):
                     s = b % 2
                     tsl = slice(b * TB, (b + 1) * TB)
                     tt(DVE, OI[s], IT[:, tsl].unsqueeze(1).to_broadcast([128, 128, TB]), iorep, ALU.is_equal,
                        [B_IT, B_iorep], [B_OI[s]])
                     tt(DVE, OJ[s], JT[:, tsl].unsqueeze(1).to_broadcast([128, 128, TB]), iorep, ALU.is_equal,
                        [B_JT, B_iorep], [B_OJ[s]])
                     tt(DVE, OI[s], OI[s], gT[:, tsl].unsqueeze(1).to_broadcast([128, 128, TB]), ALU.mult,
                        [B_OI[s], B_gT], [B_OI[s]])
                     for tl in range(TB):
                         t = b * TB + tl
                         bk = 4 + (t // 4) % 2
                         g4v = banks[bk][:].rearrange("p (i t) -> p i t", t=4)
                         mm(g4v[:, :, t % 4], OJ[s][:, :, tl], OI[s][:, :, tl], True, True, [B_OI[s], B_OJ[s]],
                            [B_bank[bk]])
                         if t % 4 == 3:
                             cp(ACT, G[:, :, t - 3:t + 1], g4v, [B_bank[bk]], [B_G])
                 chk(7)
                 LA = 3

                 def u_stage(i):
                     gi, ii = divmod(i, NI)
                     s = gi % NBUF
                     if ii == 0:
                         dma(SP, ubuf[s], uT_s[:, :, gi * NI * 128:(gi + 1) * NI * 128], [B_uTs], [B_ubuf[s]])
                         dma(SP, vbuf[s],
                             v_s[gi * NI * 128:(gi + 1) * NI * 128, :].rearrange("(i p) d -> p i d", p=128),
                             [B_vs], [B_vbuf[s]])
                     bk = 4 + i % 4
                     for kc in range(8):
                         mm(banks[bk][:, :TC], ubuf[s][:, kc, ii * 128:(ii + 1) * 128], aT[:, kc, :],
                            kc == 0, kc == 7, [B_ubuf[s], B_aT], [B_bank[bk]])
                     r4 = i % 4
                     act(actb[r4], banks[bk][:, :TC], AF.Gelu_apprx_tanh, [B_bank[bk]], [B_actb[r4]])
                     tt(DVE, gab[r4], actb[r4], G[:, i, :], ALU.mult, [B_actb[r4], B_G], [B_gab[r4]])

                 def v_stage(i):
                     gi, ii = divmod(i, NI)
                     s = gi % NBUF
                     r4 = i % 4
                     for tsub in range(2):
                         for dh in range(2):
                             ob = tsub * 2 + dh
                             mm(banks[ob][:, :], gab[r4][:, tsub * 128:(tsub + 1) * 128],
                                vbuf[s][:, ii, dh * 512:(dh + 1) * 512], i == 0, i == 127,
                                [B_gab[r4], B_vbuf[s]], [B_bank[ob]])

                 for i in range(LA):
                     u_stage(i)
                 for i in range(128):
                     if i + LA < 128:
                         u_stage(i + LA)
                     v_stage(i)
                 chk(8)
                 P.barrier()
                 arena.off = oi_off
                 pe_sb = arena.alloc([D], F32)
                 h3 = arena.alloc([D], F32)
                 junk = arena.alloc([D], F32)
                 ssq = arena.alloc([2], F32)
                 gfin = arena.alloc([D], F32)
                 osb = arena.alloc([D], F32)
                 B_pe, B_h3, B_junk, B_ssq, B_gfin, B_osb = (Buf("pe"), Buf("h3"), Buf("junk"), Buf("ssq"),
                                                            Buf("gfin"), Buf("osb"))
                 dma(SP, gfin, gfin_d, [], [B_gfin])
                 for tsub in range(2):
                     for dh in range(2):
                         cp(ACT, pe_sb[:, dh * 512:(dh + 1) * 512], banks[tsub * 2 + dh][:, :],
                            [B_bank[tsub * 2 + dh]], [B_pe])
                     if dbg:
                         r0 = t0 + c_t0 + tsub * 128
                         dma(SP, dbg_d["pe"][r0:r0 + 128, :], pe_sb, [B_pe], [])
                     for half in range(2):
                         bk = 4 + half
                         for j in range(4):
                             kc = half * 4 + j
                             tr(banks[bk][:, j * 128:(j + 1) * 128],
                                hT[:, kc, c_t0 + tsub * 128:c_t0 + (tsub + 1) * 128], ident32[:],
                                [B_hT, B_const], [B_bank[bk]])
                         tt(DVE, h3[:, half * 512:(half + 1) * 512], banks[bk][:, :],
                            pe_sb[:, half * 512:(half + 1) * 512], ALU.add, [B_bank[bk], B_pe], [B_h3])
                     act(junk, h3, AF.Square, [B_h3], [B_junk, B_ssq], accum=ssq[:, 0:1])
                     act(ssq[:, 1:2], ssq[:, 0:1], AF.Sqrt, [B_ssq], [B_ssq], bias=EPS, scale=1.0 / D)
                     P.op(DVE, lambda e: e.reciprocal(ssq[:, 0:1], ssq[:, 1:2]), reads=[B_ssq], writes=[B_ssq])
                     stt(DVE, osb, h3, ssq[:, 0:1], gfin, ALU.mult, ALU.mult, [B_h3, B_ssq, B_gfin], [B_osb])
                     r0 = t0 + c_t0 + tsub * 128
                     dma(SP, out_d[r0:r0 + 128, :], osb, [B_osb], [])

    except _Stop:
        pass
    P.emit()
    return nc


def _tables():
    half = 32
    inv = (10000.0 ** (-np.arange(half, dtype=np.float32) / half)).astype(np.float32)
    ang = np.arange(SEQ, dtype=np.float32)[None, :] * inv[:, None]
    cos, sin = np.cos(ang).astype(np.float32), np.sin(ang).astype(np.float32)
    cos64 = np.concatenate([cos, cos], 0)
    sin64 = np.concatenate([-sin, sin], 0)
    cosT = np.concatenate([cos64, cos64], 0)
    sinT = np.concatenate([sin64, sin64], 0)
    blk = np.arange(SEQ) // 256
    n = np.arange(8)
    past = (n[None, :] < blk[:, None]).astype(np.float32)
    own = (n[None, :] == blk[:, None]).astype(np.float32)
    neg = (past - 1.0) * 1e30
    rep = lambda a: np.ascontiguousarray(np.tile(a, (1, 8)).astype(np.float32))
    tri = (np.arange(128)[:, None] <= np.arange(128)[None, :]).astype(np.float32)
    eb = np.zeros((8, 8, 128), np.float32)
    for b in range(8):
        eb[b, b, :] = 1.0
    return dict(cosT=np.ascontiguousarray(cosT), sinT=np.ascontiguousarray(sinT), past64=rep(past),
                neg64=rep(neg), own64=rep(own), tri=tri, eb=eb.reshape(8, 1024),
                ident=np.eye(128, dtype=np.float32),
                iota128=np.ascontiguousarray(np.tile(np.arange(128, dtype=np.float32), (128, 1))),
                iota16=np.ascontiguousarray(np.tile(np.arange(16, dtype=np.float32), (128, 1))),
                iorep=np.ascontiguousarray(np.tile(np.repeat(np.arange(128, dtype=np.float32), 16), (128, 1))))


def _shared_inputs(inp):
    f = lambda a: np.ascontiguousarray(np.asarray(a, dtype=np.float32))
    w_in = f(inp["w_in"][0])
    perm = np.arange(512).reshape(8, 2, 32)[:, ::-1, :].reshape(512)
    w_in_ext = np.concatenate([w_in, w_in[:, 1536 + perm], w_in[:, 2048 + perm]], axis=1)
    col = lambda g: f(g).reshape(8, 128).T
    gtab = np.concatenate([col(inp["g_mix"][0]), col(inp["g_xattn"][0]), col(inp["g_mem"][0]),
                           col(inp["g_ffn"][0])], axis=1)
    wconvT = f(inp["w_conv"][0]).T.reshape(4, 128, 3).transpose(1, 0, 2).reshape(128, 12)
    d = dict(w_in_ext=f(w_in_ext), w_conv_out=f(inp["w_conv_out"][0]), w_attn_out=f(inp["w_attn_out"][0]),
             w_merge=f(inp["w_merge"][0]), w_xq=f(inp["w_xq"][0]), w_xkv=f(inp["w_xkv"][0]),
             w_xo=f(inp["w_xo"][0]), w_pq=f(inp["w_pq"][0]),
             sub_keys=f(inp["peer_sub_keys"][0]).reshape(2048, 128),
             peer_u=f(inp["peer_u"][0]), peer_v=f(inp["peer_v"][0]),
             gtab=f(gtab), gfin_bc=f(np.broadcast_to(f(inp["g_final"])[None, :], (128, D))),
             wconvT=f(wconvT))
    d.update(_tables())
    return d


def kernel(**inputs):
    n_cores = 8
    x = np.asarray(inputs["x"], dtype=np.float32)
    mem = np.asarray(inputs["mem"], dtype=np.float32)
    B = x.shape[0]
    NB = B // n_cores
    shared = _shared_inputs(inputs)
    nc = build_nc(NB)
    in_maps = []
    for c in range(n_cores):
        m = dict(shared)
        m["x"] = np.ascontiguousarray(x[c * NB:(c + 1) * NB].reshape(NB * SEQ, D))
        m["mem"] = np.ascontiguousarray(mem[c * NB:(c + 1) * NB].reshape(NB * NMEM, D))
        in_maps.append(m)
    res = run_bass_kernel_spmd(nc, in_maps, core_ids=list(range(n_cores)))
    out = np.concatenate([np.asarray(r["out"]).reshape(NB, SEQ, D) for r in res.results], axis=0)
    return out.astype(np.float32)
```
